# Optimizing a Trainium2 kernel written in Bass

```python
import jax, jax.numpy as jnp
from jax import lax
import numpy as np

D_MODEL = 1024
BATCH = 32
SEQ = 2048
DEPTH = 1

D_MIX = D_MODEL
D_GMLP = D_MIX // 2
GMLP_GROUPS = 4
GMLP_GROUP_DIM = D_GMLP // GMLP_GROUPS
GMLP_CHUNK = 128
D_HGRN = D_MIX - D_GMLP
HGRN_HEADS = 4
HGRN_DK = D_HGRN // HGRN_HEADS
HGRN_DV = D_HGRN // HGRN_HEADS
HGRN_CHUNK = 64
D_IN_PROJ = 2 * D_GMLP + 4 * D_HGRN
N_MEM = 256
XATTN_HEADS = 4
XATTN_HEAD_DIM = D_MODEL // XATTN_HEADS
N_GROUPS = 4
EXPERTS_PER_GROUP = 8
N_EXPERTS = N_GROUPS * EXPERTS_PER_GROUP
TOP_K_IN_GROUP = 2
D_EXPERT = D_MODEL // 2
EXPERT_BLOCK = 256
EPS = 1e-6

kernel_name = "hybrid_gmlp_hgrn2_xattn_hmoe"


def rms_norm(x, gain):
    x32 = x.astype(jnp.float32)
    y = x32 * lax.rsqrt(jnp.mean(x32 * x32, axis=-1, keepdims=True) + EPS)
    return (y * gain.astype(jnp.float32)).astype(x.dtype)


def layer_norm(x, gain):
    x32 = x.astype(jnp.float32)
    mu = jnp.mean(x32, axis=-1, keepdims=True)
    xc = x32 - mu
    y = xc * lax.rsqrt(jnp.mean(xc * xc, axis=-1, keepdims=True) + EPS)
    return (y * gain.astype(jnp.float32)).astype(x.dtype)


def gmlp_heads(u, v, ln_gain, w_s, b_s, beta):
    B, S, _ = u.shape
    n_chunks = S // GMLP_CHUNK
    u = jax.nn.gelu(u)
    v = layer_norm(jax.nn.gelu(v), ln_gain)
    v = v.reshape(B, n_chunks, GMLP_CHUNK, GMLP_GROUPS, GMLP_GROUP_DIM)
    causal = jnp.tril(jnp.ones((GMLP_CHUNK, GMLP_CHUNK), dtype=bool))
    w = jnp.where(causal[None], w_s, jnp.zeros((), w_s.dtype))
    z = jnp.einsum('gts,bnsgc->bntgc', w, v) + b_s.T[:, :, None]
    y = u * z.reshape(B, S, D_GMLP)
    return rms_norm(y, beta)


def hgrn2_chunked(q, k, v, log_f):
    B, S, H, Dk = q.shape
    Dv = v.shape[-1]
    n_chunks = S // HGRN_CHUNK

    def to_chunks(t):
        return t.reshape(B, n_chunks, HGRN_CHUNK, H, t.shape[-1]).transpose(1, 0, 3, 2, 4)

    q, k, v, log_f = to_chunks(q), to_chunks(k), to_chunks(v), to_chunks(log_f)
    b = jnp.cumsum(log_f, axis=-2)
    b_last = b[..., -1:, :]
    q_dec = q * jnp.exp(b)
    k_inv = k * jnp.exp(-b)
    k_to_end = k * jnp.exp(b_last - b)
    causal = jnp.tril(jnp.ones((HGRN_CHUNK, HGRN_CHUNK), dtype=bool))
    scores = jnp.einsum('nbhid,nbhjd->nbhij', q_dec, k_inv)
    scores = jnp.where(causal, scores, jnp.zeros((), scores.dtype))
    o_intra = jnp.einsum('nbhij,nbhjv->nbhiv', scores, v)

    def step(state, inp):
        q_c, k_c, v_c, decay_c = inp
        o_c = jnp.einsum('bhid,bhdv->bhiv', q_c, state)
        state = decay_c[..., 0, :, None] * state + jnp.einsum('bhjd,bhjv->bhdv', k_c, v_c)
        return state, o_c

    state0 = jnp.zeros((B, H, Dk, Dv), jnp.float32)
    _, o_inter = lax.scan(step, state0, (q_dec, k_to_end, v, jnp.exp(b_last)))
    o = o_intra + o_inter
    return o.transpose(1, 0, 3, 2, 4).reshape(B, S, H, Dv)


def hgrn2_heads(q, f_logit, i, g, lower_bound, out_gain):
    B, S, _ = q.shape
    f32 = jnp.float32
    qk_shape = (B, S, HGRN_HEADS, HGRN_DK)
    qh = jax.nn.silu(q.astype(f32)).reshape(qk_shape)
    f = lower_bound + (1.0 - lower_bound) * jax.nn.sigmoid(f_logit.astype(f32))
    kh = (1.0 - f).reshape(qk_shape)
    log_f = jnp.log(f).reshape(qk_shape)
    vh = i.astype(f32).reshape(B, S, HGRN_HEADS, HGRN_DV)
    o = hgrn2_chunked(qh, kh, vh, log_f)
    o = o * lax.rsqrt(jnp.mean(o * o, axis=-1, keepdims=True) + EPS)
    o = o.reshape(B, S, D_HGRN) * out_gain.astype(f32) * jax.nn.silu(g.astype(f32))
    return o.astype(g.dtype)


def memory_cross_attention(h, mem, w_q, w_kv, w_o):
    B, S, _ = h.shape
    M = mem.shape[1]
    q = (h @ w_q).reshape(B, S, XATTN_HEADS, XATTN_HEAD_DIM)
    k, v = jnp.split(mem @ w_kv, 2, axis=-1)
    k = k.reshape(B, M, XATTN_HEADS, XATTN_HEAD_DIM)
    v = v.reshape(B, M, XATTN_HEADS, XATTN_HEAD_DIM)
    s = jnp.einsum('bshd,bmhd->bhsm', q, k).astype(jnp.float32) * (XATTN_HEAD_DIM ** -0.5)
    p = jax.nn.softmax(s, axis=-1).astype(v.dtype)
    o = jnp.einsum('bhsm,bmhd->bshd', p, v).reshape(B, S, D_MODEL)
    return o @ w_o


def routed_expert_ffn(xt, expert_ids, gates, w_gate, w_up, w_down):
    T, K = expert_ids.shape
    A = T * K
    n_blocks = -(-A // EXPERT_BLOCK) + N_EXPERTS
    flat_e = expert_ids.reshape(A)
    order = jnp.argsort(flat_e)
    sorted_e = flat_e[order]
    counts = jnp.zeros((N_EXPERTS,), jnp.int32).at[flat_e].add(1)
    padded = (counts + EXPERT_BLOCK - 1) // EXPERT_BLOCK * EXPERT_BLOCK
    seg_start = jnp.cumsum(counts) - counts
    pad_end = jnp.cumsum(padded)
    pad_start = pad_end - padded
    dest_sorted = pad_start[sorted_e] + (jnp.arange(A, dtype=jnp.int32) - seg_start[sorted_e])
    slot_token = jnp.full((n_blocks * EXPERT_BLOCK,), T, jnp.int32).at[dest_sorted].set(
        (order // K).astype(jnp.int32))
    x_pad = jnp.concatenate([xt, jnp.zeros((1, xt.shape[1]), xt.dtype)], axis=0)
    xb = x_pad[slot_token].reshape(n_blocks, EXPERT_BLOCK, xt.shape[1])
    block_start = jnp.arange(n_blocks, dtype=jnp.int32) * EXPERT_BLOCK
    block_e = jnp.minimum(jnp.searchsorted(pad_end, block_start, side='right'), N_EXPERTS - 1)

    def expert_block(args):
        xb_i, e = args
        hid = jax.nn.silu(xb_i @ w_gate[e]) * (xb_i @ w_up[e])
        return hid @ w_down[e]

    yb = lax.map(expert_block, (xb, block_e)).reshape(n_blocks * EXPERT_BLOCK, xt.shape[1])
    dest = jnp.zeros((A,), jnp.int32).at[order].set(dest_sorted)
    y = yb[dest].reshape(T, K, xt.shape[1])
    return jnp.einsum('tk,tkd->td', gates.astype(y.dtype), y)


def hierarchical_moe(x, w_rg, b_rg, w_re, b_re, w_gate, w_up, w_down):
    B, S, D = x.shape
    T = B * S
    f32 = jnp.float32
    xt = x.reshape(T, D)
    g_logits = (xt @ w_rg).astype(f32) + b_rg.astype(f32)
    g_prob = jax.nn.softmax(g_logits, axis=-1)
    _, g_sel = lax.top_k(g_logits, 1)
    g_gate = jnp.take_along_axis(g_prob, g_sel, axis=-1)
    e_logits = ((xt @ w_re).astype(f32) + b_re.astype(f32)).reshape(T, N_GROUPS, EXPERTS_PER_GROUP)
    e_in_group = jnp.take_along_axis(e_logits, g_sel[:, :, None], axis=1)[:, 0]
    top_vals, top_idx = lax.top_k(e_in_group, TOP_K_IN_GROUP)
    gates = jax.nn.softmax(top_vals, axis=-1) * g_gate
    expert_ids = g_sel * EXPERTS_PER_GROUP + top_idx
    y = routed_expert_ffn(xt, expert_ids, gates, w_gate, w_up, w_down)
    return y.reshape(B, S, D)


def setup_inputs(seed: int = 0) -> dict:
    key = jax.random.key(seed)
    ks = jax.random.split(key, 26)
    f32 = jnp.float32

    def normal(k, shape, scale):
        return jax.random.normal(k, shape, f32) * scale

    def gain(k, shape):
        return 1.0 + 0.02 * jax.random.normal(k, shape, f32)

    L = DEPTH
    return {
        "x": normal(ks[0], (BATCH, SEQ, D_MODEL), 1.0),
        "mem": normal(ks[1], (BATCH, N_MEM, D_MODEL), 1.0),
        "norm_mix": gain(ks[2], (L, D_MODEL)),
        "w_in": normal(ks[3], (L, D_MODEL, D_IN_PROJ), D_MODEL ** -0.5),
        "gmlp_ln": gain(ks[4], (L, D_GMLP)),
        "gmlp_w_spatial": normal(ks[5], (L, GMLP_GROUPS, GMLP_CHUNK, GMLP_CHUNK), GMLP_CHUNK ** -0.5),
        "gmlp_b_spatial": gain(ks[6], (L, GMLP_GROUPS, GMLP_CHUNK)),
        "gmlp_beta": gain(ks[7], (L, D_GMLP)),
        "hgrn_lb_logits": normal(ks[8], (L + 1, D_HGRN), 0.1),
        "hgrn_out_gain": gain(ks[9], (L, D_HGRN)),
        "w_out": normal(ks[10], (L, D_MIX, D_MODEL), D_MIX ** -0.5),
        "norm_xattn": gain(ks[11], (L, D_MODEL)),
        "norm_mem": gain(ks[12], (L, D_MODEL)),
        "w_xq": normal(ks[13], (L, D_MODEL, D_MODEL), D_MODEL ** -0.5),
        "w_xkv": normal(ks[14], (L, D_MODEL, 2 * D_MODEL), D_MODEL ** -0.5),
        "w_xo": normal(ks[15], (L, D_MODEL, D_MODEL), D_MODEL ** -0.5),
        "norm_ffn": gain(ks[16], (L, D_MODEL)),
        "w_router_group": normal(ks[17], (L, D_MODEL, N_GROUPS), D_MODEL ** -0.5),
        "b_router_group": normal(ks[18], (L, N_GROUPS), 0.01),
        "w_router_expert": normal(ks[19], (L, D_MODEL, N_EXPERTS), D_MODEL ** -0.5),
        "b_router_expert": normal(ks[20], (L, N_EXPERTS), 0.01),
        "w_expert_gate": normal(ks[21], (L, N_EXPERTS, D_MODEL, D_EXPERT), D_MODEL ** -0.5),
        "w_expert_up": normal(ks[22], (L, N_EXPERTS, D_MODEL, D_EXPERT), D_MODEL ** -0.5),
        "w_expert_down": normal(ks[23], (L, N_EXPERTS, D_EXPERT, D_MODEL), D_EXPERT ** -0.5),
        "norm_final": gain(ks[24], (D_MODEL,)),
    }


def reference(x, mem, norm_mix, w_in, gmlp_ln, gmlp_w_spatial, gmlp_b_spatial, gmlp_beta,
              hgrn_lb_logits, hgrn_out_gain, w_out, norm_xattn, norm_mem, w_xq, w_xkv, w_xo,
              norm_ffn, w_router_group, b_router_group, w_router_expert, b_router_expert,
              w_expert_gate, w_expert_up, w_expert_down, norm_final):
    lower_bounds = jnp.cumsum(jax.nn.softmax(hgrn_lb_logits.astype(jnp.float32), axis=0), axis=0)
    split_at = [D_GMLP, 2 * D_GMLP, 2 * D_GMLP + D_HGRN, 2 * D_GMLP + 2 * D_HGRN,
                2 * D_GMLP + 3 * D_HGRN]
    h = x
    for l in range(DEPTH):
        a = rms_norm(h, norm_mix[l])
        u, v, q, f_logit, i, g = jnp.split(a @ w_in[l], split_at, axis=-1)
        y_a = gmlp_heads(u, v, gmlp_ln[l], gmlp_w_spatial[l], gmlp_b_spatial[l], gmlp_beta[l])
        y_b = hgrn2_heads(q, f_logit, i, g, lower_bounds[l], hgrn_out_gain[l])
        h = h + jnp.concatenate([y_a, y_b], axis=-1) @ w_out[l]
        h = h + memory_cross_attention(rms_norm(h, norm_xattn[l]), rms_norm(mem, norm_mem[l]),
                                       w_xq[l], w_xkv[l], w_xo[l])
        h = h + hierarchical_moe(rms_norm(h, norm_ffn[l]), w_router_group[l], b_router_group[l],
                                 w_router_expert[l], b_router_expert[l], w_expert_gate[l],
                                 w_expert_up[l], w_expert_down[l])
    return rms_norm(h, norm_final)
```

```python
import numpy as np
import concourse.bass as bass
import concourse.mybir as mybir
from concourse.bass_utils import run_bass_kernel_spmd

F32 = mybir.dt.float32
BF16 = mybir.dt.bfloat16
I32 = mybir.dt.int32
AF = mybir.ActivationFunctionType
ALU = mybir.AluOpType
AX = mybir.AxisListType

N_CORES = 8
D = 1024
EPS = 1e-6
NEXP = 32


class Buf:
    __slots__ = ("name", "w", "r")

    def __init__(self, name=""):
        self.name = name
        self.w = None
        self.r = {}


class Eng:
    def __init__(self, key, eng, sem):
        self.key = key
        self.eng = eng
        self.sem = sem
        self.cnt = 0
        self.pend = False
        self.seen = {}


class K:
    def __init__(self, nc, ndma_sems=8):
        self.nc = nc
        self.sems = {}
        self.stack = []

        def mk(name):
            cm = nc.semaphore("s_" + name)
            s = cm.__enter__()
            self.stack.append(cm)
            self.sems[name] = s
            return s
        self.pe = Eng("pe", nc.tensor, mk("pe"))
        self.act = Eng("act", nc.scalar, mk("act"))
        self.dve = Eng("dve", nc.vector, mk("dve"))
        self.pool = Eng("pool", nc.gpsimd, mk("pool"))
        self.sp = Eng("sp", nc.sync, mk("sp"))
        self.engs = [self.pe, self.act, self.dve, self.pool, self.sp]
        self.dpool = {}
        for q in ("sp", "pool", "act"):
            lst = []
            for i in range(ndma_sems):
                nm = "d_%s%d" % (q, i)
                mk(nm)
                lst.append([nm, 0])
            self.dpool[q] = [lst, 0]

    def close(self):
        for cm in reversed(self.stack):
            cm.__exit__(None, None, None)

    def _deps(self, E, reads, writes):
        deps = {}

        def add(ev, same_ok):
            if ev is None:
                return
            key, val = ev
            if key == E.key and not same_ok:
                return
            if deps.get(key, 0) < val:
                deps[key] = val
        for b in reads:
            add(b.w, True)
        for b in writes:
            add(b.w, False)
            for key, val in b.r.items():
                add((key, val), False)
        return deps

    def _wait(self, E, deps):
        for key, val in deps.items():
            if E.seen.get(key, 0) < val:
                if key == E.key and val > E.cnt:
                    raise RuntimeError("same-engine wait on un-inc'd event")
                E.eng.wait_ge(self.sems[key], val)
                E.seen[key] = val

    def op(self, E, fn, reads=(), writes=(), inc=True):
        self._wait(E, self._deps(E, reads, writes))
        inst = fn()
        if inc:
            E.cnt += 1
            inst.then_inc(E.sem, 1)
            E.pend = False
            val = E.cnt
        else:
            assert E is self.pe
            E.pend = True
            val = E.cnt + 1
        for b in reads:
            if b.r.get(E.key, 0) < val:
                b.r[E.key] = val
        for b in writes:
            b.w = (E.key, val)
            b.r = {}
        return inst

    def dma(self, Q, fn, reads=(), writes=()):
        self._wait(Q, self._deps(Q, reads, writes))
        lst, idx = self.dpool[Q.key]
        slot = lst[idx % len(lst)]
        self.dpool[Q.key][1] = idx + 1
        nm, cur = slot
        if cur > 0 and Q.seen.get(nm, 0) < cur:
            Q.eng.wait_ge(self.sems[nm], cur)
            Q.seen[nm] = cur
        inst = fn()
        cur += 16
        slot[1] = cur
        inst.then_inc(self.sems[nm], 16)
        for b in reads:
            if b.r.get(nm, 0) < cur:
                b.r[nm] = cur
        for b in writes:
            b.w = (nm, cur)
            b.r = {}
        return inst

    def barrier(self):
        targets = {}
        for E in self.engs:
            assert not E.pend
            if E.cnt:
                targets[E.key] = E.cnt
        for q, (lst, _) in self.dpool.items():
            for nm, cur in lst:
                if cur:
                    targets[nm] = cur
        for E in self.engs:
            for key, val in targets.items():
                if key == E.key:
                    continue
                if E.seen.get(key, 0) < val:
                    E.eng.wait_ge(self.sems[key], val)
                    E.seen[key] = val


class T:
    __slots__ = ("t", "b")

    def __init__(self, t, name):
        self.t = t
        self.b = Buf(name)


def build_nc(NSEQ, S, CAP, dbg=False):
    NT = S // 128
    TT = NSEQ * NT
    NSLOT = NEXP * CAP
    nc = bass.Bass("TRN2", target_bir_lowering=False)

    def din(name, shape, dt=F32):
        return nc.dram_tensor(name, list(shape), dt, kind="ExternalInput").ap()

    x_d = din("x", [NSEQ * S, D])
    mem_d = din("mem", [NSEQ * 256, D])
    w_in_d = din("w_in", [D, 3072])
    w_out_d = din("w_out", [D, D])
    w_xq_d = din("w_xq", [D, D])
    w_xkv_d = din("w_xkv", [D, 2 * D])
    w_xo_d = din("w_xo", [D, D])
    w_eg_d = din("w_eg", [NEXP, D, 512])
    w_eu_d = din("w_eu", [NEXP, D, 512])
    w_ed_d = din("w_ed", [NEXP, 512, D])
    w_r_d = din("w_r", [D, 68])
    b_r_d = din("b_r", [1, 68])
    gcols_d = din("gcols", [128, 32])
    lng_d = din("lng", [1, 512])
    lbl_d = din("lbl", [2, 512])
    gffn_d = din("gffn", [1, D])
    gfin_d = din("gfin", [1, D])
    wspT_d = din("wspT", [128, 4, 128])
    bspT_d = din("bspT", [128, 4])
    cst_d = din("cst", [128, 674])
    out_d = nc.dram_tensor("out", [NSEQ * S, D], F32, kind="ExternalOutput").ap()
    if dbg:
        dbg_h1 = nc.dram_tensor("dbg_h1", [NSEQ * S, D], F32, kind="ExternalOutput").ap()
        dbg_h2 = nc.dram_tensor("dbg_h2", [NSEQ * S, D], F32, kind="ExternalOutput").ap()
        dbg_yc = nc.dram_tensor("dbg_yc", [NSEQ * S, D], F32, kind="ExternalOutput").ap()
    xs_d = nc.dram_tensor("xs_scr", [NSLOT, D], BF16, kind="Internal").ap()
    yb_d = nc.dram_tensor("yb_scr", [NSLOT, D], BF16, kind="Internal").ap()
    hs_d = nc.dram_tensor("hs_scr", [TT * 128, D], F32, kind="Internal").ap()
    kv_d = nc.dram_tensor("kv_scr", [NSEQ, 128, 4096], BF16, kind="Internal").ap()

    k = K(nc)
    pe, act, dve, pool, sp = k.pe, k.act, k.dve, k.pool, k.sp
    V_, S_, G_, TE = nc.vector, nc.scalar, nc.gpsimd, nc.tensor

    scopes = [[]]

    def sb(name, shape, dt):
        cm = nc.sbuf_tensor("sb_" + name, list(shape), dt)
        t = cm.__enter__()
        scopes[-1].append(cm)
        return T(t, name)

    def sbn(name, shape, dt, n):
        return [sb("%s%d" % (name, i), shape, dt) for i in range(n)]

    def push_scope():
        scopes.append([])

    def pop_scope():
        for cm in reversed(scopes.pop()):
            cm.__exit__(None, None, None)

    banks = []
    for i in range(8):
        cm = nc.psum_tensor("ps%d" % i, [128, 512], F32)
        banks.append(T(cm.__enter__(), "ps%d" % i))
        scopes[0].append(cm)
    bank_i = [0]

    def ps():
        b = banks[bank_i[0] % 8]
        bank_i[0] += 1
        return b

    cstp = sb("cstp", [128, 162], F32)
    ident = sb("ident", [128, 128], BF16)
    ones_bf = sb("ones_bf", [128, 128], BF16)
    slt_bf = sb("slt_bf", [128, 128], BF16)
    dest = sb("dest", [128, TT, 2], I32)
    gates = sb("gates", [128, TT, 2], F32)
    nhalf = sb("nhalf", [128, 8], F32)
    C_ID, C_LE, C_LT, C_ONE, C_MX, C_EC = 0, 128, 256, 384, 512, 642
    mext = cstp.t[:, 0:130]
    ecap = cstp.t[:, 130:162]
    cst = cstp

    k.dma(sp, lambda: nc.sync.dma_start(out=cstp.t[:], in_=cst_d[:, C_MX:C_MX + 162]), writes=[cstp.b])
    k.op(pool, lambda: G_.memset(nhalf.t[:], -0.5), [], [nhalf.b])
    k.op(pool, lambda: G_.memset(nhalf.t[:, 7:8], -1.0), [], [nhalf.b])

    def rsqrt_col(dst, dst_buf, src, src_buf, scale, ncol=1):
        k.op(pool, lambda: G_.tensor_scalar(out=dst, in0=src, scalar1=scale, scalar2=EPS, op0=ALU.mult, op1=ALU.add),
             [src_buf], [dst_buf])
        k.op(pool, lambda: G_.tensor_tensor(out=dst, in0=dst, in1=nhalf.t[:, 0:ncol], op=ALU.pow),
             [dst_buf, nhalf.b], [dst_buf])

    push_scope()
    w_in = sb("w_in", [128, 8, 3072], BF16)
    w_out = sb("w_out", [128, 8, 1024], BF16)
    w_xq = sb("w_xq", [128, 8, 1024], BF16)
    w_xo = sb("w_xo", [128, 8, 1024], BF16)
    w_r = sb("w_r", [128, 8, 68], BF16)
    gcols = sb("gcols", [128, 32], F32)
    lng_b = sb("lng_b", [128, 512], F32)
    lb_b = sb("lb_b", [128, 512], F32)
    omlb_b = sb("omlb_b", [128, 512], F32)
    gffn_b = sb("gffn_b", [128, D], F32)
    br_b = sb("br_b", [128, 68], F32)
    wsT = sb("wsT", [128, 4, 128], BF16)
    bspT = sb("bspT", [128, 4], F32)
    negm4 = sb("negm4", [128, 4, 128], BF16)
    Macc = sb("Macc", [128, 32], BF16)
    Sst = sb("Sst", [128, 512], F32)

    k.dma(sp, lambda: nc.sync.dma_start(out=gcols.t[:], in_=gcols_d[:, :]), writes=[gcols.b])
    k.dma(sp, lambda: nc.sync.dma_start(out=lng_b.t[:], in_=lng_d[0:1, :].partition_broadcast(128)), writes=[lng_b.b])
    k.dma(sp, lambda: nc.sync.dma_start(out=lb_b.t[:], in_=lbl_d[0:1, :].partition_broadcast(128)), writes=[lb_b.b])
    k.dma(sp, lambda: nc.sync.dma_start(out=omlb_b.t[:], in_=lbl_d[1:2, :].partition_broadcast(128)), writes=[omlb_b.b])
    k.dma(sp, lambda: nc.sync.dma_start(out=gffn_b.t[:], in_=gffn_d[0:1, :].partition_broadcast(128)), writes=[gffn_b.b])
    k.dma(sp, lambda: nc.sync.dma_start(out=br_b.t[:], in_=b_r_d[0:1, :].partition_broadcast(128)), writes=[br_b.b])
    k.dma(sp, lambda: nc.sync.dma_start(out=bspT.t[:], in_=bspT_d[:, :]), writes=[bspT.b])
    k.dma(pool, lambda: G_.dma_start(out=w_r.t[:], in_=w_r_d.rearrange("(kc p) n -> p kc n", p=128)), writes=[w_r.b])
    k.dma(pool, lambda: G_.dma_start(out=w_xo.t[:], in_=w_xo_d.rearrange("(kc p) n -> p kc n", p=128)), writes=[w_xo.b])
    k.op(dve, lambda: V_.tensor_tensor(out=lb_b.t[:], in0=lb_b.t[:], in1=omlb_b.t[:], op=ALU.subtract), [lb_b.b, omlb_b.b], [lb_b.b])
    k.op(act, lambda: S_.activation(out=lb_b.t[:], in_=lb_b.t[:], func=AF.Sigmoid), [lb_b.b], [lb_b.b])
    k.op(dve, lambda: V_.tensor_scalar(out=omlb_b.t[:], in0=lb_b.t[:], scalar1=-1.0, scalar2=1.0, op0=ALU.mult, op1=ALU.add),
         [lb_b.b], [omlb_b.b])
    k.op(pool, lambda: G_.memset(Macc.t[:], 0.0), [], [Macc.b])
    k.op(pool, lambda: G_.memset(Sst.t[:], 0.0), [], [Sst.b])

    push_scope()
    cstb = sb("cstb", [128, 512], F32)
    k.dma(sp, lambda: nc.sync.dma_start(out=cstb.t[:], in_=cst_d[:, 0:512]), writes=[cstb.b])
    k.op(dve, lambda: V_.tensor_copy(out=ident.t[:], in_=cstb.t[:, C_ID:C_ID + 128]), [cstb.b], [ident.b])
    k.op(dve, lambda: V_.tensor_copy(out=ones_bf.t[:], in_=cstb.t[:, C_ONE:C_ONE + 128]), [cstb.b], [ones_bf.b])
    k.op(dve, lambda: V_.tensor_copy(out=slt_bf.t[:], in_=cstb.t[:, C_LT:C_LT + 128]), [cstb.b], [slt_bf.b])
    for h in range(4):
        k.op(dve, lambda h=h: V_.tensor_scalar(out=negm4.t[:, h, :], in0=cstb.t[:, C_LE:C_LE + 128], scalar1=-1.0, scalar2=None, op0=ALU.mult),
             [cstb.b], [negm4.b])
    stg = sbn("stg", [128, 2048], F32, 2)
    stg_i = [0]

    def load_scaled(dst, dst_w, src_d, ncols, gc0):
        npc = 1 if ncols <= 2048 else 2
        pw = ncols // npc
        for kc in range(8):
            for pc in range(npc):
                st = stg[stg_i[0] % 2]
                stg_i[0] += 1
                c0 = pc * pw
                k.dma(sp, lambda: nc.sync.dma_start(out=st.t[:, 0:pw], in_=src_d[kc * 128:(kc + 1) * 128, c0:c0 + pw]), writes=[st.b])
                if stg_i[0] % 2 == 0:
                    k.op(dve, lambda: V_.tensor_scalar(out=dst.t[:, kc, c0:c0 + pw], in0=st.t[:, 0:pw], scalar1=gcols.t[:, gc0 + kc:gc0 + kc + 1], scalar2=None, op0=ALU.mult),
                         [st.b, gcols.b], [dst.b])
                else:
                    k.op(act, lambda: S_.activation(out=dst.t[:, kc, c0:c0 + pw], in_=st.t[:, 0:pw], func=AF.Copy, scale=gcols.t[:, gc0 + kc:gc0 + kc + 1]),
                         [st.b, gcols.b], [dst.b])
                yield

    w_xkv = sb("w_xkv", [128, 8, 2048], BF16)
    for _ in load_scaled(w_xkv, None, w_xkv_d, 2048, 16):
        pass

    def gen_weights():
        yield from load_scaled(w_in, None, w_in_d, 3072, 0)
        yield from load_scaled(w_out, None, w_out_d, 1024, 24)
        yield from load_scaled(w_xq, None, w_xq_d, 1024, 8)

    wsf = sb("wsf", [128, 4, 128], F32)
    k.dma(sp, lambda: nc.sync.dma_start(out=wsf.t[:], in_=wspT_d[:, :, :]), writes=[wsf.b])
    for g in range(4):
        k.op(dve, lambda g=g: V_.tensor_tensor(out=wsT.t[:, g, :], in0=wsf.t[:, g, :], in1=cstb.t[:, C_LE:C_LE + 128], op=ALU.mult),
             [wsf.b, cstb.b], [wsT.b])

    def sumsq(src_ap, src_buf, junk, col_ap, col_buf, n=D):
        k.op(act, lambda: S_.activation(out=junk.t[:, 0:n], in_=src_ap, func=AF.Square, accum_out=col_ap),
             [src_buf], [junk.b, col_buf])

    mt = sbn("memt", [128, D], F32, 2)
    mjunk = sb("mjunk", [128, D], BF16)
    mn = sbn("memn", [128, D], BF16, 2)
    mnT = sb("memnT", [128, 8, 256], BF16)
    mcol = sbn("mcol", [128, 1], F32, 2)
    kvs = sbn("kvs", [128, 4096], BF16, 1)
    kv_db = [Buf("kvd%d" % s) for s in range(NSEQ)]
    def gen_kv():
      for s in range(NSEQ):
          kvt = kvs[0]
          for mc in range(2):
              m_ = mt[mc]
              k.dma(pool, lambda m_=m_, mc=mc, s=s: G_.dma_start(out=m_.t[:], in_=mem_d[s * 256 + mc * 128: s * 256 + (mc + 1) * 128, :]), writes=[m_.b])
              sumsq(m_.t[:], m_.b, mjunk, mcol[mc].t[:, 0:1], mcol[mc].b)
              rsqrt_col(mcol[mc].t[:, 0:1], mcol[mc].b, mcol[mc].t[:, 0:1], mcol[mc].b, 1.0 / D)
              k.op(dve, lambda m_=m_, mc=mc: V_.tensor_scalar(out=mn[mc].t[:], in0=m_.t[:], scalar1=mcol[mc].t[:, 0:1], scalar2=None, op0=ALU.mult),
                   [m_.b, mcol[mc].b], [mn[mc].b])
              bk = ps()
              pv = bk.t[:].bitcast(BF16)
              for kc in range(8):
                  k.op(pe, lambda kc=kc, pv=pv, mc=mc: TE.transpose(out=pv[:, kc * 128:(kc + 1) * 128], in_=mn[mc].t[:, kc * 128:(kc + 1) * 128], identity=ident.t[:]),
                       [mn[mc].b, ident.b], [bk.b], inc=(kc == 7))
              k.op(act, lambda pv=pv, mc=mc: S_.copy(out=mnT.t[:, :, mc * 128:(mc + 1) * 128], in_=pv.rearrange("p (a b) -> p a b", a=8)),
                   [bk.b], [mnT.b])
              yield
          for c in range(8):
              bk = ps()
              for kc in range(8):
                  k.op(pe, lambda kc=kc, c=c, bk=bk: TE.matmul(bk.t[:, 0:256], lhsT=w_xkv.t[:, kc, c * 128:(c + 1) * 128], rhs=mnT.t[:, kc, :], start=(kc == 0), stop=(kc == 7)),
                       [w_xkv.b, mnT.b], [bk.b], inc=(kc == 7))
              if c % 2 == 0:
                  k.op(dve, lambda c=c, bk=bk, kvt=kvt: V_.tensor_copy(out=kvt.t[:, c * 256:(c + 1) * 256], in_=bk.t[:, 0:256]), [bk.b], [kvt.b])
              else:
                  k.op(act, lambda c=c, bk=bk, kvt=kvt: S_.copy(out=kvt.t[:, c * 256:(c + 1) * 256], in_=bk.t[:, 0:256]), [bk.b], [kvt.b])
              yield
          for mc in range(2):
              for hf in range(2):
                  bk = ps()
                  for kc in range(8):
                      k.op(pe, lambda kc=kc, mc=mc, hf=hf, bk=bk: TE.matmul(bk.t[:, :], lhsT=mnT.t[:, kc, mc * 128:(mc + 1) * 128], rhs=w_xkv.t[:, kc, 1024 + hf * 512:1024 + (hf + 1) * 512], start=(kc == 0), stop=(kc == 7)),
                           [w_xkv.b, mnT.b], [bk.b], inc=(kc == 7))
                  o0 = 2048 + mc * 1024 + hf * 512
                  if hf == 0:
                      k.op(dve, lambda bk=bk, kvt=kvt, o0=o0: V_.tensor_copy(out=kvt.t[:, o0:o0 + 512], in_=bk.t[:, :]), [bk.b], [kvt.b])
                  else:
                      k.op(act, lambda bk=bk, kvt=kvt, o0=o0: S_.copy(out=kvt.t[:, o0:o0 + 512], in_=bk.t[:, :]), [bk.b], [kvt.b])
                  yield
          k.dma(sp, lambda s=s, kvt=kvt: nc.sync.dma_start(out=kv_d[s, :, :], in_=kvt.t[:]), reads=[kvt.b], writes=[kv_db[s]])
    gens_ = [gen_weights(), gen_kv()]
    while gens_:
        for g_ in list(gens_):
            try:
                next(g_)
            except StopIteration:
                gens_.remove(g_)
    k.barrier()
    pop_scope()

    NX = 5
    Xs = sbn("X", [128, D], F32, NX)
    xnA = sbn("xnA", [128, D], BF16, 1)
    xnTA = sbn("xnTA", [128, 8, 128], BF16, 1)
    xnC = sbn("xnC", [128, D], BF16, 1)
    xnTC = sbn("xnTC", [128, 8, 128], BF16, 1)
    xnD = sbn("xnD", [128, D], BF16, 1)
    xnTD = sbn("xnTD", [128, 8, 128], BF16, 1)
    ncol = sbn("ncol", [128, 16], F32, 8)
    ug = sbn("ug", [128, 512], F32, 2)
    vg = sbn("vg", [128, 512], F32, 1)
    vn = sbn("vn", [128, 512], BF16, 2)
    vst = sbn("vst", [128, 8], F32, 2)
    fS = sbn("fS", [128, 512], F32, 1)
    logf = sbn("logf", [128, 512], F32, 1)
    kinv = sbn("kinv", [128, 512], BF16, 2)
    kinvT = sbn("kinvT", [128, 512], BF16, 2)
    sq = sbn("sq", [128, 512], F32, 1)
    ebT = sbn("ebT", [128, 512], F32, 1)
    cde = sbn("cde", [128, 12], F32, 2)
    qdT = sbn("qdT", [128, 512], BF16, 2)
    vb = sbn("vb", [128, 512], BF16, 2)
    gs = sbn("gs", [128, 512], F32, 2)
    scT = sbn("scT", [128, 512], BF16, 2)
    Sp = sbn("Sp", [128, 512], BF16, 2)
    yy = sbn("yy", [128, 512], F32, 1)
    ycat = sbn("ycat", [128, D], BF16, 2)
    ycatT = sbn("ycatT", [128, 8, 128], BF16, 1)
    qTa = sbn("qTa", [128, 8, 128], BF16, 2)
    ET = sbn("ET", [128, 2, 4, 128], BF16, 1)
    Rinv = sbn("Rinv", [128, 4, 128], F32, 1)
    oT = sbn("oT", [128, 8, 128], BF16, 1)
    lg = sbn("lg", [128, 68], F32, 2)
    rt = sbn("rt", [128, 160], F32, 2)
    Mb = sbn("Mb", [128, 32], BF16, 2)
    kvr = sbn("kvr", [128, 4096], BF16, 1)

    dest_b = [Buf("dest%d" % i) for i in range(TT)]
    gates_b = [Buf("gates%d" % i) for i in range(TT)]
    x_db = Buf("x_dram")
    hs_db = [Buf("hs%d" % i) for i in range(TT)]
    xs_db = [Buf("xs%d" % i) for i in range(2 * TT)]
    dbg_db = Buf("dbg")

    def mm_group(bk, out_ap, pairs, reads):
        n = len(pairs)
        for j, (l, r) in enumerate(pairs):
            k.op(pe, lambda l=l, r=r, j=j: TE.matmul(out_ap, lhsT=l, rhs=r, start=(j == 0), stop=(j == n - 1)),
                 reads, [bk.b], inc=(j == n - 1))

    def dbg_dump(src, src_buf, dst_d, i, bf=False):
        if not dbg:
            return
        if bf:
            return
            d_ = dbt[i % 2]
            k.op(dve, lambda: V_.tensor_copy(out=d_.t[:], in_=src), [src_buf], [d_.b])
            k.dma(sp, lambda: nc.sync.dma_start(out=dst_d[i * 128:(i + 1) * 128, :], in_=d_.t[:]), reads=[d_.b], writes=[dbg_db])
        else:
            k.dma(sp, lambda: nc.sync.dma_start(out=dst_d[i * 128:(i + 1) * 128, :], in_=src), reads=[src_buf], writes=[dbg_db])

    stage_banks = {"A": [0, 1, 2], "B": [3, 4], "C1": [5], "C2": [6], "D": [7]}
    stage_bi = {kk: 0 for kk in stage_banks}

    def psS(st):
        lst = stage_banks[st]
        b = banks[lst[stage_bi[st] % len(lst)]]
        stage_bi[st] += 1
        return b

    def norm_T(st, X, xn_t, xnT_t, col, gain=None):
        sumsq(X.t[:], X.b, xn_t, col.t[:, 0:1], col.b)
        rsqrt_col(col.t[:, 0:1], col.b, col.t[:, 0:1], col.b, 1.0 / D)
        yield
        if gain is None:
            k.op(dve, lambda: V_.tensor_scalar(out=xn_t.t[:], in0=X.t[:], scalar1=col.t[:, 0:1], scalar2=None, op0=ALU.mult),
                 [X.b, col.b], [xn_t.b])
        else:
            k.op(dve, lambda: V_.scalar_tensor_tensor(out=xn_t.t[:], in0=X.t[:], scalar=col.t[:, 0:1], in1=gain.t[:], op0=ALU.mult, op1=ALU.mult),
                 [X.b, col.b, gain.b], [xn_t.b])
        yield
        bk = psS(st)
        pv = bk.t[:].bitcast(BF16)
        for kc in range(8):
            k.op(pe, lambda kc=kc: TE.transpose(out=pv[:, kc * 128:(kc + 1) * 128], in_=xn_t.t[:, kc * 128:(kc + 1) * 128], identity=ident.t[:]),
                 [xn_t.b, ident.b], [bk.b], inc=(kc == 7))
        yield
        k.op(act, lambda: S_.copy(out=xnT_t.t[:].rearrange("p a b -> p (a b)"), in_=pv), [bk.b], [xnT_t.b])
        yield

    def stage_A(i):
        X = Xs[i % NX]
        s2 = i % 2
        k.dma(sp, lambda: nc.sync.dma_start(out=X.t[:], in_=x_d[i * 128:(i + 1) * 128, :]), reads=[x_db], writes=[X.b])
        col = ncol[i % 8]
        xT = xnTA[0]
        yield from norm_T("A", X, xnA[0], xT, col)
        ug_, vg_, vn_, fS_, lf_, vb_, gs_, sq_ = ug[s2], vg[0], vn[s2], fS[0], logf[0], vb[s2], gs[s2], sq[0]

        def tok_group(c):
            bk = psS("A")
            mm_group(bk, bk.t[:, :], [(xT.t[:, kc, :], w_in.t[:, kc, c * 512:(c + 1) * 512]) for kc in range(8)], [xT.b, w_in.b])
            return bk
        b_f = tok_group(3)
        yield
        b_i = tok_group(4)
        k.op(act, lambda: S_.activation(out=fS_.t[:], in_=b_f.t[:, :], func=AF.Sigmoid), [b_f.b], [fS_.b])
        yield
        b_v = tok_group(1)
        k.op(dve, lambda: V_.tensor_copy(out=vb_.t[:], in_=b_i.t[:, :]), [b_i.b], [vb_.b])
        k.op(pool, lambda: G_.tensor_tensor(out=fS_.t[:], in0=fS_.t[:], in1=omlb_b.t[:], op=ALU.mult), [fS_.b, omlb_b.b], [fS_.b])
        k.op(pool, lambda: G_.tensor_tensor(out=fS_.t[:], in0=fS_.t[:], in1=lb_b.t[:], op=ALU.add), [fS_.b, lb_b.b], [fS_.b])
        yield
        b_u = tok_group(0)
        k.op(act, lambda: S_.activation(out=vg_.t[:], in_=b_v.t[:, :], func=AF.Gelu_apprx_tanh), [b_v.b], [vg_.b])
        yield
        b_g = tok_group(5)
        k.op(act, lambda: S_.activation(out=ug_.t[:], in_=b_u.t[:, :], func=AF.Gelu_apprx_tanh), [b_u.b], [ug_.b])
        k.op(act, lambda: S_.activation(out=lf_.t[:], in_=fS_.t[:], func=AF.Ln), [fS_.b], [lf_.b])
        st_ = vst[s2]
        k.op(dve, lambda: V_.bn_stats(out=st_.t[:, 0:6], in_=vg_.t[:]), [vg_.b], [st_.b])
        k.op(dve, lambda: V_.bn_aggr(out=col.t[:, 2:4], in_=st_.t[:, 0:6]), [st_.b], [col.b])
        rsqrt_col(col.t[:, 3:4], col.b, col.t[:, 3:4], col.b, 1.0)
        yield
        b_q = psS("A")
        for h in range(4):
            mm_group(b_q, b_q.t[:, h * 128:(h + 1) * 128],
                     [(w_in.t[:, kc, 1024 + h * 128:1024 + (h + 1) * 128], xT.t[:, kc, :]) for kc in range(8)], [xT.b, w_in.b])
        k.op(act, lambda: S_.activation(out=gs_.t[:], in_=b_g.t[:, :], func=AF.Silu), [b_g.b], [gs_.b])
        k.op(dve, lambda: V_.scalar_tensor_tensor(out=col.t[:, 4:5], in0=col.t[:, 2:3], scalar=-1.0, in1=col.t[:, 3:4], op0=ALU.mult, op1=ALU.mult),
             [col.b], [col.b])
        yield
        b_b = psS("A")
        k.op(pe, lambda: TE.matmul(b_b.t[:, :], lhsT=mext[:, 0:128], rhs=lf_.t[:, :], start=True, stop=True), [cst.b, lf_.b], [b_b.b])
        k.op(act, lambda: S_.activation(out=sq_.t[:], in_=b_q.t[:, :], func=AF.Silu), [b_q.b], [sq_.b])
        k.op(act, lambda: S_.activation(out=vg_.t[:], in_=vg_.t[:], func=AF.Identity, scale=col.t[:, 3:4], bias=col.t[:, 4:5]),
             [vg_.b, col.b], [vg_.b])
        yield
        b_bT = psS("A")
        for h in range(4):
            k.op(pe, lambda h=h: TE.matmul(b_bT.t[:, h * 128:(h + 1) * 128], lhsT=lf_.t[:, h * 128:(h + 1) * 128], rhs=mext[:, 0:128], start=True, stop=True),
                 [cst.b, lf_.b], [b_bT.b], inc=(h == 3))
        k.op(dve, lambda: V_.tensor_tensor(out=vn_.t[:], in0=vg_.t[:], in1=lng_b.t[:], op=ALU.mult), [vg_.b, lng_b.b], [vn_.b])
        yield
        b_cd = psS("A")
        for h in range(4):
            k.op(pe, lambda h=h: TE.matmul(b_cd.t[:, h * 2:(h + 1) * 2], lhsT=lf_.t[:, h * 128:(h + 1) * 128], rhs=mext[:, 128:130], start=True, stop=True),
                 [cst.b, lf_.b], [b_cd.b], inc=(h == 3))
        en_, ki_, kT_, eb_, cd_, qd_ = logf[0], kinv[s2], kinvT[s2], ebT[0], cde[s2], qdT[s2]
        k.op(act, lambda: S_.activation(out=eb_.t[:], in_=b_bT.t[:, :], func=AF.Exp), [b_bT.b], [eb_.b])
        yield
        k.op(act, lambda: S_.activation(out=cd_.t[:, 0:8], in_=b_cd.t[:, 0:8], func=AF.Exp), [b_cd.b], [cd_.b])
        k.op(act, lambda: S_.activation(out=en_.t[:], in_=b_b.t[:, :], func=AF.Exp, scale=-1.0), [b_b.b], [en_.b])
        k.op(dve, lambda: V_.tensor_copy(out=cd_.t[:, 8:12], in_=eb_.t[:].rearrange("p (h t) -> p h t", h=4)[:, :, 127]), [eb_.b], [cd_.b])
        k.op(dve, lambda: V_.tensor_tensor(out=qd_.t[:], in0=sq_.t[:], in1=eb_.t[:], op=ALU.mult), [sq_.b, eb_.b], [qd_.b])
        yield
        k.op(dve, lambda: V_.scalar_tensor_tensor(out=ki_.t[:], in0=fS_.t[:], scalar=1.0, in1=en_.t[:], op0=ALU.subtract, op1=ALU.mult),
             [fS_.b, en_.b], [ki_.b])
        yield
        bk = psS("A")
        pv = bk.t[:].bitcast(BF16)
        for h in range(4):
            k.op(pe, lambda h=h: TE.transpose(out=pv[:, h * 128:(h + 1) * 128], in_=ki_.t[:, h * 128:(h + 1) * 128], identity=ident.t[:]),
                 [ki_.b, ident.b], [bk.b], inc=(h == 3))
        yield
        k.op(act, lambda: S_.copy(out=kT_.t[:], in_=pv[:, 0:512]), [bk.b], [kT_.b])

    def stage_B(i):
        s2 = i % 2
        col = ncol[i % 8]
        ug_, vn_, vb_, gs_ = ug[s2], vn[s2], vb[s2], gs[s2]
        ki_, kT_, cd_, qd_ = kinv[s2], kinvT[s2], cde[s2], qdT[s2]
        sc_, Sp_, yc_, yy_, ut_ = scT[s2], Sp[s2], ycat[s2], yy[0], yy[0]
        if i % NT == 0 and i > 0:
            k.op(pool, lambda: G_.memset(Sst.t[:], 0.0), [], [Sst.b])
        b_s = psS("B")
        for h in range(4):
            k.op(pe, lambda h=h: TE.matmul(b_s.t[:, h * 128:(h + 1) * 128], lhsT=kT_.t[:, h * 128:(h + 1) * 128], rhs=qd_.t[:, h * 128:(h + 1) * 128], start=True, stop=True),
                 [kT_.b, qd_.b], [b_s.b], inc=(h == 3))
        b_z = psS("B")
        for g in range(4):
            k.op(pe, lambda g=g: TE.matmul(b_z.t[:, g * 128:(g + 1) * 128], lhsT=wsT.t[:, g, :], rhs=vn_.t[:, g * 128:(g + 1) * 128], start=True, stop=True),
                 [wsT.b, vn_.b], [b_z.b], inc=(g == 3))
        v4 = lambda ap: ap.rearrange("p (h v) -> p h v", h=4)
        cdv = cd_.t[:, 0:8].rearrange("p (h two) -> p h two", two=2)
        k.op(dve, lambda: V_.tensor_tensor(out=v4(Sp_.t[:]), in0=v4(Sst.t[:]), in1=cdv[:, :, 0:1].to_broadcast([128, 4, 128]), op=ALU.mult),
             [Sst.b, cd_.b], [Sp_.b])
        yield
        k.op(dve, lambda: V_.tensor_tensor(out=sc_.t[:], in0=b_s.t[:, :], in1=negm4.t[:].rearrange("p a b -> p (a b)"), op=ALU.mult),
             [b_s.b, negm4.b], [sc_.b])
        for g in range(4):
            k.op(dve, lambda g=g: V_.scalar_tensor_tensor(out=yy_.t[:, g * 128:(g + 1) * 128], in0=b_z.t[:, g * 128:(g + 1) * 128], scalar=bspT.t[:, g:g + 1],
                                                          in1=ug_.t[:, g * 128:(g + 1) * 128], op0=ALU.add, op1=ALU.mult),
                 [b_z.b, bspT.b, ug_.b], [yy_.b])
        yield
        b_o = psS("B")
        for h in range(4):
            hs_ = slice(h * 128, (h + 1) * 128)
            k.op(pe, lambda hs_=hs_: TE.matmul(b_o.t[:, hs_], lhsT=sc_.t[:, hs_], rhs=vb_.t[:, hs_], start=True, stop=False),
                 [sc_.b, vb_.b], [b_o.b], inc=False)
            k.op(pe, lambda hs_=hs_: TE.matmul(b_o.t[:, hs_], lhsT=qd_.t[:, hs_], rhs=Sp_.t[:, hs_], start=False, stop=True),
                 [qd_.b, Sp_.b], [b_o.b], inc=(h == 3))
        b_U = psS("B")
        for h in range(4):
            hs_ = slice(h * 128, (h + 1) * 128)
            k.op(pe, lambda hs_=hs_: TE.matmul(b_U.t[:, hs_], lhsT=ki_.t[:, hs_], rhs=vb_.t[:, hs_], start=True, stop=True),
                 [ki_.b, vb_.b], [b_U.b], inc=(h == 3))
        sumsq(yy_.t[:], yy_.b, yc_, col.t[:, 5:6], col.b, n=512)
        rsqrt_col(col.t[:, 5:6], col.b, col.t[:, 5:6], col.b, 1.0 / 512)
        yield
        k.op(dve, lambda: V_.tensor_scalar(out=yc_.t[:, 0:512], in0=yy_.t[:], scalar1=col.t[:, 5:6], scalar2=None, op0=ALU.mult),
             [yy_.b, col.b], [yc_.b])
        for h in range(4):
            hs_ = slice(h * 128, (h + 1) * 128)
            k.op(act, lambda hs_=hs_, h=h: S_.activation(out=yc_.t[:, 512 + h * 128:512 + (h + 1) * 128], in_=b_o.t[:, hs_], func=AF.Square, accum_out=col.t[:, 8 + h:9 + h]),
                 [b_o.b], [yc_.b, col.b])
        rsqrt_col(col.t[:, 8:12], col.b, col.t[:, 8:12], col.b, 1.0 / 128, ncol=4)
        yield
        k.op(dve, lambda: V_.tensor_tensor(out=v4(yy_.t[:]), in0=v4(b_o.t[:, :]), in1=col.t[:, 8:12].unsqueeze(2).to_broadcast([128, 4, 128]), op=ALU.mult),
             [b_o.b, col.b], [yy_.b])
        k.op(dve, lambda: V_.tensor_tensor(out=yc_.t[:, 512:1024], in0=yy_.t[:], in1=gs_.t[:], op=ALU.mult), [yy_.b, gs_.b], [yc_.b])
        k.op(dve, lambda: V_.tensor_tensor(out=v4(ut_.t[:]), in0=v4(b_U.t[:, :]), in1=cd_.t[:, 8:12].unsqueeze(2).to_broadcast([128, 4, 128]), op=ALU.mult),
             [b_U.b, cd_.b], [ut_.b])
        k.op(dve, lambda: V_.tensor_tensor(out=v4(Sst.t[:]), in0=v4(Sst.t[:]), in1=cdv[:, :, 1:2].to_broadcast([128, 4, 128]), op=ALU.mult),
             [Sst.b, cd_.b], [Sst.b])
        k.op(dve, lambda: V_.tensor_tensor(out=Sst.t[:], in0=Sst.t[:], in1=ut_.t[:], op=ALU.subtract), [Sst.b, ut_.b], [Sst.b])

    def stage_C(i):
        X = Xs[i % NX]
        s2 = i % 2
        col = ncol[i % 8]
        yc_, ycT_ = ycat[s2], ycatT[0]
        bk = psS("C1")
        pv = bk.t[:].bitcast(BF16)
        for kc in range(8):
            k.op(pe, lambda kc=kc: TE.transpose(out=pv[:, kc * 128:(kc + 1) * 128], in_=yc_.t[:, kc * 128:(kc + 1) * 128], identity=ident.t[:]),
                 [yc_.b, ident.b], [bk.b], inc=(kc == 7))
        yield
        k.op(act, lambda: S_.copy(out=ycT_.t[:].rearrange("p a b -> p (a b)"), in_=pv), [bk.b], [ycT_.b])
        yield
        for hf in range(2):
            bk = psS("C1")
            mm_group(bk, bk.t[:, :], [(ycT_.t[:, kc, :], w_out.t[:, kc, hf * 512:(hf + 1) * 512]) for kc in range(8)], [ycT_.b, w_out.b])
            yield
            k.op(dve, lambda hf=hf, bk=bk: V_.tensor_tensor(out=X.t[:, hf * 512:(hf + 1) * 512], in0=X.t[:, hf * 512:(hf + 1) * 512], in1=bk.t[:, :], op=ALU.add),
                 [X.b, bk.b], [X.b])
        dbg_dump(X.t[:], X.b, dbg_h1 if dbg else None, i)
        xT = xnTC[0]
        yield from norm_T("C1", X, xnC[0], xT, col)
        qT_ = qTa[s2]
        for half in range(2):
            bk = psS("C1")
            for cc in range(4):
                c = half * 4 + cc
                mm_group(bk, bk.t[:, cc * 128:(cc + 1) * 128], [(w_xq.t[:, kc, c * 128:(c + 1) * 128], xT.t[:, kc, :]) for kc in range(8)], [xT.b, w_xq.b])
            yield
            if half == 0:
                k.op(act, lambda bk=bk: S_.copy(out=qT_.t[:, 0:4, :].rearrange("p a b -> p (a b)"), in_=bk.t[:, :]), [bk.b], [qT_.b])
            else:
                k.op(dve, lambda bk=bk: V_.tensor_copy(out=qT_.t[:, 4:8, :].rearrange("p a b -> p (a b)"), in_=bk.t[:, :]), [bk.b], [qT_.b])

    def stage_C2(i):
        X = Xs[i % NX]
        s2 = i % 2
        kv_ = kvr[0]
        qT_ = qTa[s2]
        if i % NT == 0:
            s = i // NT
            k.dma(sp, lambda: nc.sync.dma_start(out=kv_.t[:], in_=kv_d[s, :, :]), reads=[kv_db[s]], writes=[kv_.b])
        ET_ = ET[0]
        for mc in range(2):
            bk = psS("C2")
            for h in range(4):
                mm_group(bk, bk.t[:, h * 128:(h + 1) * 128],
                         [(kv_.t[:, (2 * h + dc) * 256 + mc * 128:(2 * h + dc) * 256 + (mc + 1) * 128], qT_.t[:, 2 * h + dc, :]) for dc in range(2)], [kv_.b, qT_.b])
            yield
            k.op(act, lambda bk=bk, mc=mc: S_.activation(out=ET_.t[:, mc, :, :].rearrange("p a b -> p (a b)"), in_=bk.t[:, :], func=AF.Exp, scale=1.0 / 16.0),
                 [bk.b], [ET_.b])
        yield
        b_r = psS("C2")
        mm_group(b_r, b_r.t[:, :], [(ones_bf.t[:], ET_.t[:, mc, :, :].rearrange("p a b -> p (a b)")) for mc in range(2)], [ones_bf.b, ET_.b])
        yield
        Ri = Rinv[0]
        k.op(dve, lambda: V_.reciprocal(out=Ri.t[:].rearrange("p a b -> p (a b)"), in_=b_r.t[:, :]), [b_r.b], [Ri.b])
        oT_ = oT[0]
        for half in range(2):
            bk = psS("C2")
            for cc in range(4):
                c = half * 4 + cc
                h, dc = c // 2, c % 2
                mm_group(bk, bk.t[:, cc * 128:(cc + 1) * 128],
                         [(kv_.t[:, 2048 + mc * 1024 + h * 256 + dc * 128:2048 + mc * 1024 + h * 256 + (dc + 1) * 128], ET_.t[:, mc, h, :]) for mc in range(2)], [kv_.b, ET_.b])
            yield
            k.op(dve, lambda bk=bk, half=half: V_.tensor_tensor(out=oT_.t[:, 4 * half:4 * half + 4, :].rearrange("p (h c) t -> p h c t", h=2),
                                                                in0=bk.t[:, :].rearrange("p (h c t) -> p h c t", h=2, c=2),
                                                                in1=Ri.t[:, 2 * half:2 * half + 2, :].unsqueeze(2).to_broadcast([128, 2, 2, 128]), op=ALU.mult),
                 [bk.b, Ri.b], [oT_.b])
        yield
        for hf in range(2):
            bk = psS("C2")
            mm_group(bk, bk.t[:, :], [(oT_.t[:, c, :], w_xo.t[:, c, hf * 512:(hf + 1) * 512]) for c in range(8)], [oT_.b, w_xo.b])
            yield
            k.op(dve, lambda hf=hf, bk=bk: V_.tensor_tensor(out=X.t[:, hf * 512:(hf + 1) * 512], in0=X.t[:, hf * 512:(hf + 1) * 512], in1=bk.t[:, :], op=ALU.add),
                 [X.b, bk.b], [X.b])
        dbg_dump(X.t[:], X.b, dbg_h2 if dbg else None, i)

    def stage_D(i):
        X = Xs[i % NX]
        s2 = i % 2
        col = ncol[i % 8]
        xT, xn_ = xnTD[0], xnD[0]
        k.dma(sp, lambda: nc.sync.dma_start(out=hs_d[i * 128:(i + 1) * 128, :], in_=X.t[:]), reads=[X.b], writes=[hs_db[i]])
        yield from norm_T("D", X, xn_, xT, col, gain=gffn_b)
        b_l = psS("D")
        mm_group(b_l, b_l.t[:, 0:68], [(xT.t[:, kc, :], w_r.t[:, kc, :]) for kc in range(8)], [xT.b, w_r.b])
        yield
        lg_, r_, Mb_ = lg[s2], rt[s2], Mb[s2]
        R = r_.t
        k.op(dve, lambda: V_.tensor_tensor(out=lg_.t[:], in0=b_l.t[:, 0:68], in1=br_b.t[:], op=ALU.add), [b_l.b, br_b.b], [lg_.b])
        k.op(dve, lambda: V_.tensor_reduce(out=R[:, 0:1], in_=lg_.t[:, 64:68], axis=AX.X, op=ALU.max), [lg_.b], [r_.b])
        yield
        k.op(pool, lambda: G_.tensor_scalar(out=R[:, 1:2], in0=R[:, 0:1], scalar1=-1.0, scalar2=None, op0=ALU.mult), [r_.b], [r_.b])
        k.op(pool, lambda: G_.tensor_scalar(out=R[:, 32:64], in0=lg_.t[:, 32:64], scalar1=R[:, 1:2], scalar2=1e30, op0=ALU.add, op1=ALU.mult), [lg_.b, r_.b], [r_.b])
        k.op(pool, lambda: G_.tensor_tensor(out=R[:, 32:64], in0=R[:, 32:64], in1=lg_.t[:, 0:32], op=ALU.add), [r_.b, lg_.b], [r_.b])
        yield
        k.op(dve, lambda: V_.max(out=R[:, 4:12], in_=R[:, 32:64]), [r_.b], [r_.b])
        yield
        k.op(pool, lambda: G_.tensor_scalar(out=R[:, 64:96], in0=R[:, 32:64], scalar1=R[:, 4:5], scalar2=None, op0=ALU.is_equal), [r_.b], [r_.b])
        k.op(pool, lambda: G_.tensor_scalar(out=R[:, 96:128], in0=R[:, 32:64], scalar1=R[:, 5:6], scalar2=None, op0=ALU.is_equal), [r_.b], [r_.b])
        k.op(pool, lambda: G_.tensor_tensor(out=Mb_.t[:], in0=R[:, 64:96], in1=R[:, 96:128], op=ALU.add), [r_.b], [Mb_.b])
        k.op(pool, lambda: G_.tensor_tensor(out=R[:, 12:13], in0=R[:, 5:6], in1=R[:, 4:5], op=ALU.subtract), [r_.b], [r_.b])
        yield
        b_p = psS("D")
        k.op(pe, lambda: TE.matmul(b_p.t[:, 0:32], lhsT=slt_bf.t[:], rhs=Mb_.t[:], start=True, stop=False), [slt_bf.b, Mb_.b], [b_p.b], inc=False)
        k.op(pe, lambda: TE.matmul(b_p.t[:, 0:32], lhsT=ones_bf.t[:], rhs=Macc.t[:], start=False, stop=True), [ones_bf.b, Macc.b], [b_p.b])
        k.op(act, lambda: S_.activation(out=R[:, 20:24], in_=lg_.t[:, 64:68], func=AF.Exp, bias=R[:, 1:2], accum_out=R[:, 2:3]), [lg_.b, r_.b], [r_.b])
        k.op(act, lambda: S_.activation(out=R[:, 13:14], in_=R[:, 12:13], func=AF.Exp), [r_.b], [r_.b])
        yield
        k.op(dve, lambda: V_.scalar_tensor_tensor(out=R[:, 128:160], in0=b_p.t[:, 0:32], scalar=float(CAP - 1), in1=ecap, op0=ALU.min, op1=ALU.add), [b_p.b, cst.b], [r_.b])
        k.op(pool, lambda: G_.tensor_tensor(out=Macc.t[:], in0=Macc.t[:], in1=Mb_.t[:], op=ALU.add), [Macc.b, Mb_.b], [Macc.b])
        yield
        k.op(pool, lambda: G_.tensor_tensor(out=R[:, 64:96], in0=R[:, 64:96], in1=R[:, 128:160], op=ALU.mult), [r_.b], [r_.b])
        k.op(pool, lambda: G_.tensor_tensor(out=R[:, 96:128], in0=R[:, 96:128], in1=R[:, 128:160], op=ALU.mult), [r_.b], [r_.b])
        k.op(pool, lambda: G_.tensor_scalar(out=R[:, 14:15], in0=R[:, 13:14], scalar1=1.0, scalar2=R[:, 2:3], op0=ALU.add, op1=ALU.mult), [r_.b], [r_.b])
        k.op(pool, lambda: G_.tensor_tensor(out=gates.t[:, i, 0:1], in0=R[:, 14:15], in1=nhalf.t[:, 7:8], op=ALU.pow), [r_.b, nhalf.b], [gates_b[i]])
        k.op(pool, lambda: G_.tensor_tensor(out=gates.t[:, i, 1:2], in0=R[:, 13:14], in1=gates.t[:, i, 0:1], op=ALU.mult), [r_.b, gates_b[i]], [gates_b[i]])
        yield
        k.op(dve, lambda: V_.tensor_reduce(out=R[:, 16:18], in_=R[:, 64:128].rearrange("p (a b) -> p a b", a=2), axis=AX.X, op=ALU.add), [r_.b], [r_.b])
        k.op(dve, lambda: V_.tensor_scalar(out=dest.t[:, i, :], in0=R[:, 16:18], scalar1=float(NSLOT - 1), scalar2=None, op0=ALU.min), [r_.b], [dest_b[i]])
        yield
        for kk in range(2):
            k.dma(pool, lambda kk=kk: G_.indirect_dma_start(out=xs_d[:, :], out_offset=bass.IndirectOffsetOnAxis(ap=dest.t[:, i, kk:kk + 1], axis=0),
                                                           in_=xn_.t[:], in_offset=None),
                  reads=[xn_.b, dest_b[i]], writes=[xs_db[2 * i + kk]])

    stages = [stage_A, stage_B, stage_C, stage_C2, stage_D]
    nst = len(stages)
    prereq = {
        0: lambda i: [(0, i - 1), (1, i - 2), (4, i - 5)],
        1: lambda i: [(0, i), (1, i - 1), (2, i - 2)],
        2: lambda i: [(1, i), (2, i - 1), (3, i - 2)],
        3: lambda i: [(2, i), (3, i - 1)],
        4: lambda i: [(3, i), (4, i - 1)],
    }
    done = set()
    nxt = [0] * nst
    active = []
    while len(done) < nst * TT:
        for s_i in reversed(range(nst)):
            i_ = nxt[s_i]
            if i_ < TT and all((p[1] < 0) or (p in done) for p in prereq[s_i](i_)):
                active.append(((s_i, i_), stages[s_i](i_)))
                nxt[s_i] += 1
        for item in sorted(active, key=lambda it: (it[0][1], -it[0][0])):
            try:
                next(item[1])
                if item[0][0] == 4:
                    next(item[1])
            except StopIteration:
                active.remove(item)
                done.add(item[0])
    k.barrier()
    pop_scope()

    push_scope()
    wg = sbn("wg", [128, 8, 512], BF16, 2)
    wu = sbn("wu", [128, 8, 512], BF16, 2)
    wd = sbn("wd", [128, 4, 1024], BF16, 2)
    xsT = sbn("xsT", [128, 8, CAP], BF16, 2)
    hid = sbn("hid", [128, 4, CAP], BF16, 2)
    sgt = sbn("sgt", [128, 512], F32, 2)
    yo = sbn("yo", [128, D], BF16, 3)
    gfin_b = sb("gfin_b", [128, D], F32)
    hsT = sbn("hsT", [128, D], F32, 3)
    y0 = sbn("y0", [128, D], BF16, 3)
    fo = sbn("fo", [128, D], F32, 2)
    y1 = sbn("y1", [128, D], BF16, 3)
    fcol = sbn("fcol", [128, 2], F32, 3)
    epsc = sb("epsc", [128, 1], F32)
    k.op(pool, lambda: G_.memset(epsc.t[:], EPS), [], [epsc.b])
    fjunk = sb("fjunk", [128, D], BF16)
    k.dma(sp, lambda: nc.sync.dma_start(out=gfin_b.t[:], in_=gfin_d[0:1, :].partition_broadcast(128)), writes=[gfin_b.b])
    wdb = Buf("wdram")
    yb_db = Buf("yb")
    blocks = []
    o_ = 0
    while o_ < CAP:
        w_ = min(512, CAP - o_)
        blocks.append((o_, w_))
        o_ += w_
    yo_i = 0
    sg_i = 0
    NCH = CAP // 128
    xtok = sbn("xtok", [128, D], BF16, 3)
    xtok_i = [0]

    def exp_loads(e):
        se = e % 2
        wg_, wu_, wd_ = wg[se], wu[se], wd[se]
        k.dma(pool, lambda: G_.dma_start(out=wg_.t[:], in_=w_eg_d[e].rearrange("(kc p) f -> p kc f", p=128)), reads=[wdb], writes=[wg_.b])
        k.dma(pool, lambda: G_.dma_start(out=wu_.t[:], in_=w_eu_d[e].rearrange("(kc p) f -> p kc f", p=128)), reads=[wdb], writes=[wu_.b])
        k.dma(pool, lambda: G_.dma_start(out=wd_.t[:], in_=w_ed_d[e].rearrange("(fc p) d -> p fc d", p=128)), reads=[wdb], writes=[wd_.b])

    def exp_xpose(e, sc):
        xT_ = xsT[e % 2]
        xt_ = xtok[xtok_i[0] % 3]
        xtok_i[0] += 1
        r0 = e * CAP + sc * 128
        k.dma(sp, lambda: nc.sync.dma_start(out=xt_.t[:], in_=xs_d[r0:r0 + 128, :]), reads=[wdb], writes=[xt_.b])
        bk = ps()
        pv = bk.t[:].bitcast(BF16)
        for kc in range(8):
            k.op(pe, lambda kc=kc: TE.transpose(out=pv[:, kc * 128:(kc + 1) * 128], in_=xt_.t[:, kc * 128:(kc + 1) * 128], identity=ident.t[:]),
                 [xt_.b, ident.b], [bk.b], inc=(kc == 7))
        if sc % 2 == 0:
            k.op(act, lambda: S_.copy(out=xT_.t[:, :, sc * 128:(sc + 1) * 128], in_=pv.rearrange("p (a b) -> p a b", a=8)), [bk.b], [xT_.b])
        else:
            k.op(dve, lambda: V_.tensor_copy(out=xT_.t[:, :, sc * 128:(sc + 1) * 128], in_=pv.rearrange("p (a b) -> p a b", a=8)), [bk.b], [xT_.b])

    def exp_compute(e):
        nonlocal yo_i, sg_i
        se = e % 2
        wg_, wu_, wd_, xT_, hd_ = wg[se], wu[se], wd[se], xsT[se], hid[se]
        nxt_sc = 0
        for (o0, w0) in blocks:
            for fc in range(4):
                b_g, b_u = ps(), ps()
                mm_group(b_g, b_g.t[:, 0:w0], [(wg_.t[:, kc, fc * 128:(fc + 1) * 128], xT_.t[:, kc, o0:o0 + w0]) for kc in range(8)], [wg_.b, xT_.b])
                mm_group(b_u, b_u.t[:, 0:w0], [(wu_.t[:, kc, fc * 128:(fc + 1) * 128], xT_.t[:, kc, o0:o0 + w0]) for kc in range(8)], [wu_.b, xT_.b])
                sg_ = sgt[sg_i % 2]
                sg_i += 1
                k.op(act, lambda: S_.activation(out=sg_.t[:, 0:w0], in_=b_g.t[:, 0:w0], func=AF.Silu), [b_g.b], [sg_.b])
                k.op(dve, lambda: V_.tensor_tensor(out=hd_.t[:, fc, o0:o0 + w0], in0=sg_.t[:, 0:w0], in1=b_u.t[:, 0:w0], op=ALU.mult), [sg_.b, b_u.b], [hd_.b])
                if e + 1 < NEXP and nxt_sc < NCH:
                    exp_xpose(e + 1, nxt_sc)
                    nxt_sc += 1
        for sc in range(NCH):
            yo_ = yo[yo_i % 3]
            yo_i += 1
            for hf in range(2):
                bk = ps()
                mm_group(bk, bk.t[:, :], [(hd_.t[:, fc, sc * 128:(sc + 1) * 128], wd_.t[:, fc, hf * 512:(hf + 1) * 512]) for fc in range(4)], [hd_.b, wd_.b])
                if hf == 0:
                    k.op(act, lambda: S_.copy(out=yo_.t[:, 0:512], in_=bk.t[:, :]), [bk.b], [yo_.b])
                else:
                    k.op(dve, lambda: V_.tensor_copy(out=yo_.t[:, 512:1024], in_=bk.t[:, :]), [bk.b], [yo_.b])
            r0 = e * CAP + sc * 128
            k.dma(act, lambda: S_.dma_start(out=yb_d[r0:r0 + 128, :], in_=yo_.t[:]), reads=[yo_.b], writes=[Buf()])
        while e + 1 < NEXP and nxt_sc < NCH:
            exp_xpose(e + 1, nxt_sc)
            nxt_sc += 1

    exp_loads(0)
    for sc in range(NCH):
        exp_xpose(0, sc)
    for e in range(NEXP):
        if e + 1 < NEXP:
            exp_loads(e + 1)
        exp_compute(e)
    k.barrier()

    out_db = Buf("out")
    NF = 3

    def fin_loads(i):
        s3 = i % NF
        h_, a_, b_ = hsT[s3], y0[s3], y1[s3]
        k.dma(sp, lambda: nc.sync.dma_start(out=h_.t[:], in_=hs_d[i * 128:(i + 1) * 128, :]), reads=[wdb], writes=[h_.b])
        k.dma(pool, lambda: G_.indirect_dma_start(out=a_.t[:], out_offset=None, in_=yb_d[:, :], in_offset=bass.IndirectOffsetOnAxis(ap=dest.t[:, i, 0:1], axis=0)),
              reads=[wdb, dest_b[i]], writes=[a_.b])
        k.dma(pool, lambda: G_.indirect_dma_start(out=b_.t[:], out_offset=None, in_=yb_d[:, :], in_offset=bass.IndirectOffsetOnAxis(ap=dest.t[:, i, 1:2], axis=0)),
              reads=[wdb, dest_b[i]], writes=[b_.b])

    def fin_compute(i):
        s3 = i % NF
        h_, a_, b_, c_ = hsT[s3], y0[s3], y1[s3], fcol[s3]
        k.op(dve, lambda: V_.scalar_tensor_tensor(out=h_.t[:], in0=a_.t[:], scalar=gates.t[:, i, 0:1], in1=h_.t[:], op0=ALU.mult, op1=ALU.add),
             [a_.b, gates_b[i], h_.b], [h_.b])
        k.op(dve, lambda: V_.scalar_tensor_tensor(out=h_.t[:], in0=b_.t[:], scalar=gates.t[:, i, 1:2], in1=h_.t[:], op0=ALU.mult, op1=ALU.add),
             [b_.b, gates_b[i], h_.b], [h_.b])
        k.op(act, lambda: S_.activation(out=fjunk.t[:], in_=h_.t[:], func=AF.Square, accum_out=c_.t[:, 0:1]), [h_.b], [fjunk.b, c_.b])
        k.op(act, lambda: S_.activation(out=c_.t[:, 0:1], in_=c_.t[:, 0:1], func=AF.Sqrt, scale=1.0 / D, bias=epsc.t[:, 0:1]), [c_.b, epsc.b], [c_.b])
        k.op(dve, lambda: V_.reciprocal(out=c_.t[:, 0:1], in_=c_.t[:, 0:1]), [c_.b], [c_.b])
        fo_ = fo[i % 2]
        k.op(dve, lambda: V_.scalar_tensor_tensor(out=fo_.t[:], in0=h_.t[:], scalar=c_.t[:, 0:1], in1=gfin_b.t[:], op0=ALU.mult, op1=ALU.mult),
             [h_.b, c_.b, gfin_b.b], [fo_.b])
        k.dma(act, lambda: S_.dma_start(out=out_d[i * 128:(i + 1) * 128, :], in_=fo_.t[:]), reads=[fo_.b], writes=[out_db])

    fin_loads(0)
    if TT > 1:
        fin_loads(1)
    for i in range(TT):
        if i + 2 < TT:
            fin_loads(i + 2)
        fin_compute(i)
    k.barrier()
    pop_scope()
    pop_scope()
    k.close()
    return nc


def _consts(CAP):
    c = np.zeros((128, 674), np.float32)
    s = np.arange(128)[:, None]
    t = np.arange(128)[None, :]
    c[:, 0:128] = (s == t)
    c[:, 128:256] = (s <= t)
    c[:, 256:384] = (s < t)
    c[:, 384:512] = 1.0
    m = np.zeros((128, 130), np.float32)
    m[:, 0:128] = ((s >= 64) & (s <= t)).astype(np.float32) - ((s <= 63) & (s > t)).astype(np.float32)
    m[:, 128] = (np.arange(128) <= 63)
    m[:, 129] = 1.0
    c[:, 512:642] = m
    c[:, 642:674] = (np.arange(32) * CAP)[None, :]
    return c


def _prep_shared(inp, CAP):
    f = lambda a: np.ascontiguousarray(np.asarray(a, dtype=np.float32))
    col = lambda v: f(v).reshape(8, 128).T
    w_rg = f(inp["w_router_group"][0])
    w_re = f(inp["w_router_expert"][0])
    b_rg = f(inp["b_router_group"][0])
    b_re = f(inp["b_router_expert"][0])
    w_r = np.concatenate([w_re, np.repeat(w_rg, 8, axis=1), w_rg], axis=1)
    b_r = np.concatenate([b_re, np.repeat(b_rg, 8), b_rg])[None, :]
    gcols = np.concatenate([col(inp["norm_mix"][0]), col(inp["norm_xattn"][0]), col(inp["norm_mem"][0]),
                            col(np.concatenate([f(inp["gmlp_beta"][0]), f(inp["hgrn_out_gain"][0])]))], axis=1)
    return {
        "w_in": f(inp["w_in"][0]), "w_out": f(inp["w_out"][0]), "w_xq": f(inp["w_xq"][0]),
        "w_xkv": f(inp["w_xkv"][0]), "w_xo": f(inp["w_xo"][0]),
        "w_eg": f(inp["w_expert_gate"][0]), "w_eu": f(inp["w_expert_up"][0]), "w_ed": f(inp["w_expert_down"][0]),
        "w_r": f(w_r), "b_r": f(b_r), "gcols": f(gcols),
        "lng": f(inp["gmlp_ln"][0])[None, :], "lbl": f(inp["hgrn_lb_logits"]),
        "gffn": f(inp["norm_ffn"][0])[None, :], "gfin": f(inp["norm_final"])[None, :],
        "wspT": f(np.transpose(f(inp["gmlp_w_spatial"][0]), (2, 0, 1))),
        "bspT": f(f(inp["gmlp_b_spatial"][0]).T),
        "cst": _consts(CAP),
    }


CAP_FULL = 1152


def run(inp, n_cores, NSEQ, S, CAP, dbg=False):
    nc = build_nc(NSEQ, S, CAP, dbg=dbg)
    shared = _prep_shared(inp, CAP)
    x = np.asarray(inp["x"], dtype=np.float32)
    mem = np.asarray(inp["mem"], dtype=np.float32)
    in_maps = []
    for c in range(n_cores):
        m = dict(shared)
        m["x"] = np.ascontiguousarray(x[c * NSEQ:(c + 1) * NSEQ].reshape(NSEQ * S, D))
        m["mem"] = np.ascontiguousarray(mem[c * NSEQ:(c + 1) * NSEQ].reshape(NSEQ * 256, D))
        in_maps.append(m)
    res = run_bass_kernel_spmd(nc, in_maps, core_ids=list(range(n_cores)))
    return res


def kernel(**inputs):
    B, S, _ = inputs["x"].shape
    NSEQ = B // N_CORES
    res = run(inputs, N_CORES, NSEQ, S, CAP_FULL)
    out = np.concatenate([r["out"].reshape(NSEQ, S, D) for r in res.results], axis=0)
    return out.astype(np.float32)
```

```python
import numpy as np
import concourse.bass as bass
import concourse.mybir as mybir
from concourse.bass_utils import run_bass_kernel_spmd

F32 = mybir.dt.float32
BF16 = mybir.dt.bfloat16
I32 = mybir.dt.int32
AF = mybir.ActivationFunctionType
ALU = mybir.AluOpType
AX = mybir.AxisListType

N_CORES = 8
D = 1024
EPS = 1e-6
NEXP = 32


class Buf:
    __slots__ = ("name", "w", "r")

    def __init__(self, name=""):
        self.name = name
        self.w = None
        self.r = {}


class Eng:
    def __init__(self, key, eng, sem):
        self.key = key
        self.eng = eng
        self.sem = sem
        self.cnt = 0
        self.pend = False
        self.seen = {}


class K:
    def __init__(self, nc, ndma_sems=8):
        self.nc = nc
        self.sems = {}
        self.stack = []

        def mk(name):
            cm = nc.semaphore("s_" + name)
            s = cm.__enter__()
            self.stack.append(cm)
            self.sems[name] = s
            return s
        self.pe = Eng("pe", nc.tensor, mk("pe"))
        self.act = Eng("act", nc.scalar, mk("act"))
        self.dve = Eng("dve", nc.vector, mk("dve"))
        self.pool = Eng("pool", nc.gpsimd, mk("pool"))
        self.sp = Eng("sp", nc.sync, mk("sp"))
        self.engs = [self.pe, self.act, self.dve, self.pool, self.sp]
        self.dpool = {}
        for q in ("sp", "pool", "act"):
            lst = []
            for i in range(ndma_sems):
                nm = "d_%s%d" % (q, i)
                mk(nm)
                lst.append([nm, 0])
            self.dpool[q] = [lst, 0]

    def close(self):
        for cm in reversed(self.stack):
            cm.__exit__(None, None, None)

    def _deps(self, E, reads, writes):
        deps = {}

        def add(ev, same_ok):
            if ev is None:
                return
            key, val = ev
            if key == E.key and not same_ok:
                return
            if deps.get(key, 0) < val:
                deps[key] = val
        for b in reads:
            add(b.w, True)
        for b in writes:
            add(b.w, False)
            for key, val in b.r.items():
                add((key, val), False)
        return deps

    def _wait(self, E, deps):
        for key, val in deps.items():
            if E.seen.get(key, 0) < val:
                if key == E.key and val > E.cnt:
                    raise RuntimeError("same-engine wait on un-inc'd event")
                E.eng.wait_ge(self.sems[key], val)
                E.seen[key] = val

    def op(self, E, fn, reads=(), writes=(), inc=True):
        self._wait(E, self._deps(E, reads, writes))
        inst = fn()
        if inc:
            E.cnt += 1
            inst.then_inc(E.sem, 1)
            E.pend = False
            val = E.cnt
        else:
            assert E is self.pe
            E.pend = True
            val = E.cnt + 1
        for b in reads:
            if b.r.get(E.key, 0) < val:
                b.r[E.key] = val
        for b in writes:
            b.w = (E.key, val)
            b.r = {}
        return inst

    def dma(self, Q, fn, reads=(), writes=()):
        self._wait(Q, self._deps(Q, reads, writes))
        lst, idx = self.dpool[Q.key]
        slot = lst[idx % len(lst)]
        self.dpool[Q.key][1] = idx + 1
        nm, cur = slot
        if cur > 0 and Q.seen.get(nm, 0) < cur:
            Q.eng.wait_ge(self.sems[nm], cur)
            Q.seen[nm] = cur
        inst = fn()
        cur += 16
        slot[1] = cur
        inst.then_inc(self.sems[nm], 16)
        for b in reads:
            if b.r.get(nm, 0) < cur:
                b.r[nm] = cur
        for b in writes:
            b.w = (nm, cur)
            b.r = {}
        return inst

    def barrier(self):
        targets = {}
        for E in self.engs:
            assert not E.pend
            if E.cnt:
                targets[E.key] = E.cnt
        for q, (lst, _) in self.dpool.items():
            for nm, cur in lst:
                if cur:
                    targets[nm] = cur
        for E in self.engs:
            for key, val in targets.items():
                if key == E.key:
                    continue
                if E.seen.get(key, 0) < val:
                    E.eng.wait_ge(self.sems[key], val)
                    E.seen[key] = val


class T:
    __slots__ = ("t", "b")

    def __init__(self, t, name):
        self.t = t
        self.b = Buf(name)


def build_nc(NSEQ, S, CAP, dbg=False):
    NT = S // 128
    TT = NSEQ * NT
    NSLOT = NEXP * CAP
    nc = bass.Bass("TRN2", target_bir_lowering=False)

    def din(name, shape, dt=F32):
        return nc.dram_tensor(name, list(shape), dt, kind="ExternalInput").ap()

    x_d = din("x", [NSEQ * S, D])
    mem_d = din("mem", [NSEQ * 256, D])
    w_in_d = din("w_in", [D, 3072])
    w_out_d = din("w_out", [D, D])
    w_xq_d = din("w_xq", [D, D])
    w_xkv_d = din("w_xkv", [D, 2 * D])
    w_xo_d = din("w_xo", [D, D])
    w_eg_d = din("w_eg", [NEXP, D, 512])
    w_eu_d = din("w_eu", [NEXP, D, 512])
    w_ed_d = din("w_ed", [NEXP, 512, D])
    w_r_d = din("w_r", [D, 68])
    b_r_d = din("b_r", [1, 68])
    gcols_d = din("gcols", [128, 32])
    lng_d = din("lng", [1, 512])
    lbl_d = din("lbl", [2, 512])
    gffn_d = din("gffn", [1, D])
    gfin_d = din("gfin", [1, D])
    wspT_d = din("wspT", [128, 4, 128])
    bspT_d = din("bspT", [128, 4])
    cst_d = din("cst", [128, 674])
    out_d = nc.dram_tensor("out", [NSEQ * S, D], F32, kind="ExternalOutput").ap()
    if dbg:
        dbg_h1 = nc.dram_tensor("dbg_h1", [NSEQ * S, D], F32, kind="ExternalOutput").ap()
        dbg_h2 = nc.dram_tensor("dbg_h2", [NSEQ * S, D], F32, kind="ExternalOutput").ap()
        dbg_yc = nc.dram_tensor("dbg_yc", [NSEQ * S, D], F32, kind="ExternalOutput").ap()
    xs_d = nc.dram_tensor("xs_scr", [NSLOT, D], BF16, kind="Internal").ap()
    yb_d = nc.dram_tensor("yb_scr", [NSLOT, D], BF16, kind="Internal").ap()
    hs_d = nc.dram_tensor("hs_scr", [TT * 128, D], F32, kind="Internal").ap()
    kv_d = nc.dram_tensor("kv_scr", [NSEQ, 128, 4096], BF16, kind="Internal").ap()

    k = K(nc)
    pe, act, dve, pool, sp = k.pe, k.act, k.dve, k.pool, k.sp
    V_, S_, G_, TE = nc.vector, nc.scalar, nc.gpsimd, nc.tensor

    scopes = [[]]

    def sb(name, shape, dt):
        cm = nc.sbuf_tensor("sb_" + name, list(shape), dt)
        t = cm.__enter__()
        scopes[-1].append(cm)
        return T(t, name)

    def sbn(name, shape, dt, n):
        return [sb("%s%d" % (name, i), shape, dt) for i in range(n)]

    def push_scope():
        scopes.append([])

    def pop_scope():
        for cm in reversed(scopes.pop()):
            cm.__exit__(None, None, None)

    banks = []
    for i in range(8):
        cm = nc.psum_tensor("ps%d" % i, [128, 512], F32)
        banks.append(T(cm.__enter__(), "ps%d" % i))
        scopes[0].append(cm)
    bank_i = [0]

    def ps():
        b = banks[bank_i[0] % 8]
        bank_i[0] += 1
        return b

    cstp = sb("cstp", [128, 162], F32)
    ident = sb("ident", [128, 128], BF16)
    ones_bf = sb("ones_bf", [128, 128], BF16)
    slt_bf = sb("slt_bf", [128, 128], BF16)
    dest = sb("dest", [128, TT, 2], I32)
    gates = sb("gates", [128, TT, 2], F32)
    nhalf = sb("nhalf", [128, 8], F32)
    C_ID, C_LE, C_LT, C_ONE, C_MX, C_EC = 0, 128, 256, 384, 512, 642
    mext = cstp.t[:, 0:130]
    ecap = cstp.t[:, 130:162]
    cst = cstp

    k.dma(sp, lambda: nc.sync.dma_start(out=cstp.t[:], in_=cst_d[:, C_MX:C_MX + 162]), writes=[cstp.b])
    k.op(pool, lambda: G_.memset(nhalf.t[:], -0.5), [], [nhalf.b])
    k.op(pool, lambda: G_.memset(nhalf.t[:, 7:8], -1.0), [], [nhalf.b])

    def rsqrt_col(dst, dst_buf, src, src_buf, scale, ncol=1):
        k.op(pool, lambda: G_.tensor_scalar(out=dst, in0=src, scalar1=scale, scalar2=EPS, op0=ALU.mult, op1=ALU.add),
             [src_buf], [dst_buf])
        k.op(pool, lambda: G_.tensor_tensor(out=dst, in0=dst, in1=nhalf.t[:, 0:ncol], op=ALU.pow),
             [dst_buf, nhalf.b], [dst_buf])

    push_scope()
    w_in = sb("w_in", [128, 8, 3072], BF16)
    w_out = sb("w_out", [128, 8, 1024], BF16)
    w_xq = sb("w_xq", [128, 8, 1024], BF16)
    w_xo = sb("w_xo", [128, 8, 1024], BF16)
    w_r = sb("w_r", [128, 8, 68], BF16)
    gcols = sb("gcols", [128, 32], F32)
    lng_b = sb("lng_b", [128, 512], F32)
    lb_b = sb("lb_b", [128, 512], F32)
    omlb_b = sb("omlb_b", [128, 512], F32)
    gffn_b = sb("gffn_b", [128, D], F32)
    br_b = sb("br_b", [128, 68], F32)
    wsT = sb("wsT", [128, 4, 128], BF16)
    bspT = sb("bspT", [128, 4], F32)
    negm4 = sb("negm4", [128, 4, 128], BF16)
    Macc = sb("Macc", [128, 32], BF16)
    Sst = sb("Sst", [128, 512], F32)

    k.dma(sp, lambda: nc.sync.dma_start(out=gcols.t[:], in_=gcols_d[:, :]), writes=[gcols.b])
    k.dma(sp, lambda: nc.sync.dma_start(out=lng_b.t[:], in_=lng_d[0:1, :].partition_broadcast(128)), writes=[lng_b.b])
    k.dma(sp, lambda: nc.sync.dma_start(out=lb_b.t[:], in_=lbl_d[0:1, :].partition_broadcast(128)), writes=[lb_b.b])
    k.dma(sp, lambda: nc.sync.dma_start(out=omlb_b.t[:], in_=lbl_d[1:2, :].partition_broadcast(128)), writes=[omlb_b.b])
    k.dma(sp, lambda: nc.sync.dma_start(out=gffn_b.t[:], in_=gffn_d[0:1, :].partition_broadcast(128)), writes=[gffn_b.b])
    k.dma(sp, lambda: nc.sync.dma_start(out=br_b.t[:], in_=b_r_d[0:1, :].partition_broadcast(128)), writes=[br_b.b])
    k.dma(sp, lambda: nc.sync.dma_start(out=bspT.t[:], in_=bspT_d[:, :]), writes=[bspT.b])
    k.dma(pool, lambda: G_.dma_start(out=w_r.t[:], in_=w_r_d.rearrange("(kc p) n -> p kc n", p=128)), writes=[w_r.b])
    k.dma(pool, lambda: G_.dma_start(out=w_xo.t[:], in_=w_xo_d.rearrange("(kc p) n -> p kc n", p=128)), writes=[w_xo.b])
    k.op(dve, lambda: V_.tensor_tensor(out=lb_b.t[:], in0=lb_b.t[:], in1=omlb_b.t[:], op=ALU.subtract), [lb_b.b, omlb_b.b], [lb_b.b])
    k.op(act, lambda: S_.activation(out=lb_b.t[:], in_=lb_b.t[:], func=AF.Sigmoid), [lb_b.b], [lb_b.b])
    k.op(dve, lambda: V_.tensor_scalar(out=omlb_b.t[:], in0=lb_b.t[:], scalar1=-1.0, scalar2=1.0, op0=ALU.mult, op1=ALU.add),
         [lb_b.b], [omlb_b.b])
    k.op(pool, lambda: G_.memset(Macc.t[:], 0.0), [], [Macc.b])
    k.op(pool, lambda: G_.memset(Sst.t[:], 0.0), [], [Sst.b])

    push_scope()
    cstb = sb("cstb", [128, 512], F32)
    k.dma(sp, lambda: nc.sync.dma_start(out=cstb.t[:], in_=cst_d[:, 0:512]), writes=[cstb.b])
    k.op(dve, lambda: V_.tensor_copy(out=ident.t[:], in_=cstb.t[:, C_ID:C_ID + 128]), [cstb.b], [ident.b])
    k.op(dve, lambda: V_.tensor_copy(out=ones_bf.t[:], in_=cstb.t[:, C_ONE:C_ONE + 128]), [cstb.b], [ones_bf.b])
    k.op(dve, lambda: V_.tensor_copy(out=slt_bf.t[:], in_=cstb.t[:, C_LT:C_LT + 128]), [cstb.b], [slt_bf.b])
    for h in range(4):
        k.op(dve, lambda h=h: V_.tensor_scalar(out=negm4.t[:, h, :], in0=cstb.t[:, C_LE:C_LE + 128], scalar1=-1.0, scalar2=None, op0=ALU.mult),
             [cstb.b], [negm4.b])
    stg = sbn("stg", [128, 2048], F32, 2)
    stg_i = [0]

    def load_scaled(dst, dst_w, src_d, ncols, gc0):
        npc = 1 if ncols <= 2048 else 2
        pw = ncols // npc
        for kc in range(8):
            for pc in range(npc):
                st = stg[stg_i[0] % 2]
                stg_i[0] += 1
                c0 = pc * pw
                k.dma(sp, lambda: nc.sync.dma_start(out=st.t[:, 0:pw], in_=src_d[kc * 128:(kc + 1) * 128, c0:c0 + pw]), writes=[st.b])
                if stg_i[0] % 2 == 0:
                    k.op(dve, lambda: V_.tensor_scalar(out=dst.t[:, kc, c0:c0 + pw], in0=st.t[:, 0:pw], scalar1=gcols.t[:, gc0 + kc:gc0 + kc + 1], scalar2=None, op0=ALU.mult),
                         [st.b, gcols.b], [dst.b])
                else:
                    k.op(act, lambda: S_.activation(out=dst.t[:, kc, c0:c0 + pw], in_=st.t[:, 0:pw], func=AF.Copy, scale=gcols.t[:, gc0 + kc:gc0 + kc + 1]),
                         [st.b, gcols.b], [dst.b])
                yield

    w_xkv = sb("w_xkv", [128, 8, 2048], BF16)
    for _ in load_scaled(w_xkv, None, w_xkv_d, 2048, 16):
        pass

    def gen_weights():
        yield from load_scaled(w_in, None, w_in_d, 3072, 0)
        yield from load_scaled(w_out, None, w_out_d, 1024, 24)
        yield from load_scaled(w_xq, None, w_xq_d, 1024, 8)

    wsf = sb("wsf", [128, 4, 128], F32)
    k.dma(sp, lambda: nc.sync.dma_start(out=wsf.t[:], in_=wspT_d[:, :, :]), writes=[wsf.b])
    for g in range(4):
        k.op(dve, lambda g=g: V_.tensor_tensor(out=wsT.t[:, g, :], in0=wsf.t[:, g, :], in1=cstb.t[:, C_LE:C_LE + 128], op=ALU.mult),
             [wsf.b, cstb.b], [wsT.b])

    def sumsq(src_ap, src_buf, junk, col_ap, col_buf, n=D):
        k.op(act, lambda: S_.activation(out=junk.t[:, 0:n], in_=src_ap, func=AF.Square, accum_out=col_ap),
             [src_buf], [junk.b, col_buf])

    mt = sbn("memt", [128, D], F32, 2)
    mjunk = sb("mjunk", [128, D], BF16)
    mn = sbn("memn", [128, D], BF16, 2)
    mnT = sb("memnT", [128, 8, 256], BF16)
    mcol = sbn("mcol", [128, 1], F32, 2)
    kvs = sbn("kvs", [128, 4096], BF16, 1)
    kv_db = [Buf("kvd%d" % s) for s in range(NSEQ)]
    def gen_kv():
      for s in range(NSEQ):
          kvt = kvs[0]
          for mc in range(2):
              m_ = mt[mc]
              k.dma(pool, lambda m_=m_, mc=mc, s=s: G_.dma_start(out=m_.t[:], in_=mem_d[s * 256 + mc * 128: s * 256 + (mc + 1) * 128, :]), writes=[m_.b])
              sumsq(m_.t[:], m_.b, mjunk, mcol[mc].t[:, 0:1], mcol[mc].b)
              rsqrt_col(mcol[mc].t[:, 0:1], mcol[mc].b, mcol[mc].t[:, 0:1], mcol[mc].b, 1.0 / D)
              k.op(dve, lambda m_=m_, mc=mc: V_.tensor_scalar(out=mn[mc].t[:], in0=m_.t[:], scalar1=mcol[mc].t[:, 0:1], scalar2=None, op0=ALU.mult),
                   [m_.b, mcol[mc].b], [mn[mc].b])
              bk = ps()
              pv = bk.t[:].bitcast(BF16)
              for kc in range(8):
                  k.op(pe, lambda kc=kc, pv=pv, mc=mc: TE.transpose(out=pv[:, kc * 128:(kc + 1) * 128], in_=mn[mc].t[:, kc * 128:(kc + 1) * 128], identity=ident.t[:]),
                       [mn[mc].b, ident.b], [bk.b], inc=(kc == 7))
              k.op(act, lambda pv=pv, mc=mc: S_.copy(out=mnT.t[:, :, mc * 128:(mc + 1) * 128], in_=pv.rearrange("p (a b) -> p a b", a=8)),
                   [bk.b], [mnT.b])
              yield
          for c in range(8):
              bk = ps()
              for kc in range(8):
                  k.op(pe, lambda kc=kc, c=c, bk=bk: TE.matmul(bk.t[:, 0:256], lhsT=w_xkv.t[:, kc, c * 128:(c + 1) * 128], rhs=mnT.t[:, kc, :], start=(kc == 0), stop=(kc == 7)),
                       [w_xkv.b, mnT.b], [bk.b], inc=(kc == 7))
              if c % 2 == 0:
                  k.op(dve, lambda c=c, bk=bk, kvt=kvt: V_.tensor_copy(out=kvt.t[:, c * 256:(c + 1) * 256], in_=bk.t[:, 0:256]), [bk.b], [kvt.b])
              else:
                  k.op(act, lambda c=c, bk=bk, kvt=kvt: S_.copy(out=kvt.t[:, c * 256:(c + 1) * 256], in_=bk.t[:, 0:256]), [bk.b], [kvt.b])
              yield
          for mc in range(2):
              for hf in range(2):
                  bk = ps()
                  for kc in range(8):
                      k.op(pe, lambda kc=kc, mc=mc, hf=hf, bk=bk: TE.matmul(bk.t[:, :], lhsT=mnT.t[:, kc, mc * 128:(mc + 1) * 128], rhs=w_xkv.t[:, kc, 1024 + hf * 512:1024 + (hf + 1) * 512], start=(kc == 0), stop=(kc == 7)),
                           [w_xkv.b, mnT.b], [bk.b], inc=(kc == 7))
                  o0 = 2048 + mc * 1024 + hf * 512
                  if hf == 0:
                      k.op(dve, lambda bk=bk, kvt=kvt, o0=o0: V_.tensor_copy(out=kvt.t[:, o0:o0 + 512], in_=bk.t[:, :]), [bk.b], [kvt.b])
                  else:
                      k.op(act, lambda bk=bk, kvt=kvt, o0=o0: S_.copy(out=kvt.t[:, o0:o0 + 512], in_=bk.t[:, :]), [bk.b], [kvt.b])
                  yield
          k.dma(sp, lambda s=s, kvt=kvt: nc.sync.dma_start(out=kv_d[s, :, :], in_=kvt.t[:]), reads=[kvt.b], writes=[kv_db[s]])
    gens_ = [gen_weights(), gen_kv()]
    while gens_:
        for g_ in list(gens_):
            try:
                next(g_)
            except StopIteration:
                gens_.remove(g_)
    k.barrier()
    pop_scope()

    NX = 5
    Xs = sbn("X", [128, D], F32, NX)
    xnA = sbn("xnA", [128, D], BF16, 1)
    xnTA = sbn("xnTA", [128, 8, 128], BF16, 1)
    xnC = sbn("xnC", [128, D], BF16, 1)
    xnTC = sbn("xnTC", [128, 8, 128], BF16, 1)
    xnD = sbn("xnD", [128, D], BF16, 1)
    xnTD = sbn("xnTD", [128, 8, 128], BF16, 1)
    ncol = sbn("ncol", [128, 16], F32, 8)
    ug = sbn("ug", [128, 512], F32, 2)
    vg = sbn("vg", [128, 512], F32, 1)
    vn = sbn("vn", [128, 512], BF16, 2)
    vst = sbn("vst", [128, 8], F32, 2)
    fS = sbn("fS", [128, 512], F32, 1)
    logf = sbn("logf", [128, 512], F32, 1)
    kinv = sbn("kinv", [128, 512], BF16, 2)
    kinvT = sbn("kinvT", [128, 512], BF16, 2)
    sq = sbn("sq", [128, 512], F32, 1)
    ebT = sbn("ebT", [128, 512], F32, 1)
    cde = sbn("cde", [128, 12], F32, 2)
    qdT = sbn("qdT", [128, 512], BF16, 2)
    vb = sbn("vb", [128, 512], BF16, 2)
    gs = sbn("gs", [128, 512], F32, 2)
    scT = sbn("scT", [128, 512], BF16, 2)
    Sp = sbn("Sp", [128, 512], BF16, 2)
    yy = sbn("yy", [128, 512], F32, 1)
    ycat = sbn("ycat", [128, D], BF16, 2)
    ycatT = sbn("ycatT", [128, 8, 128], BF16, 1)
    qTa = sbn("qTa", [128, 8, 128], BF16, 2)
    ET = sbn("ET", [128, 2, 4, 128], BF16, 1)
    Rinv = sbn("Rinv", [128, 4, 128], F32, 1)
    oT = sbn("oT", [128, 8, 128], BF16, 1)
    lg = sbn("lg", [128, 68], F32, 2)
    rt = sbn("rt", [128, 160], F32, 2)
    Mb = sbn("Mb", [128, 32], BF16, 2)
    kvr = sbn("kvr", [128, 4096], BF16, 1)

    dest_b = [Buf("dest%d" % i) for i in range(TT)]
    gates_b = [Buf("gates%d" % i) for i in range(TT)]
    x_db = Buf("x_dram")
    hs_db = [Buf("hs%d" % i) for i in range(TT)]
    xs_db = [Buf("xs%d" % i) for i in range(2 * TT)]
    dbg_db = Buf("dbg")

    def mm_group(bk, out_ap, pairs, reads):
        n = len(pairs)
        for j, (l, r) in enumerate(pairs):
            k.op(pe, lambda l=l, r=r, j=j: TE.matmul(out_ap, lhsT=l, rhs=r, start=(j == 0), stop=(j == n - 1)),
                 reads, [bk.b], inc=(j == n - 1))

    def dbg_dump(src, src_buf, dst_d, i, bf=False):
        if not dbg:
            return
        if bf:
            return
            d_ = dbt[i % 2]
            k.op(dve, lambda: V_.tensor_copy(out=d_.t[:], in_=src), [src_buf], [d_.b])
            k.dma(sp, lambda: nc.sync.dma_start(out=dst_d[i * 128:(i + 1) * 128, :], in_=d_.t[:]), reads=[d_.b], writes=[dbg_db])
        else:
            k.dma(sp, lambda: nc.sync.dma_start(out=dst_d[i * 128:(i + 1) * 128, :], in_=src), reads=[src_buf], writes=[dbg_db])

    stage_banks = {"A": [0, 1, 2], "B": [3, 4], "C1": [5], "C2": [6], "D": [7]}
    stage_bi = {kk: 0 for kk in stage_banks}

    def psS(st):
        lst = stage_banks[st]
        b = banks[lst[stage_bi[st] % len(lst)]]
        stage_bi[st] += 1
        return b

    def norm_T(st, X, xn_t, xnT_t, col, gain=None):
        sumsq(X.t[:], X.b, xn_t, col.t[:, 0:1], col.b)
        rsqrt_col(col.t[:, 0:1], col.b, col.t[:, 0:1], col.b, 1.0 / D)
        yield
        if gain is None:
            k.op(dve, lambda: V_.tensor_scalar(out=xn_t.t[:], in0=X.t[:], scalar1=col.t[:, 0:1], scalar2=None, op0=ALU.mult),
                 [X.b, col.b], [xn_t.b])
        else:
            k.op(dve, lambda: V_.scalar_tensor_tensor(out=xn_t.t[:], in0=X.t[:], scalar=col.t[:, 0:1], in1=gain.t[:], op0=ALU.mult, op1=ALU.mult),
                 [X.b, col.b, gain.b], [xn_t.b])
        yield
        bk = psS(st)
        pv = bk.t[:].bitcast(BF16)
        for kc in range(8):
            k.op(pe, lambda kc=kc: TE.transpose(out=pv[:, kc * 128:(kc + 1) * 128], in_=xn_t.t[:, kc * 128:(kc + 1) * 128], identity=ident.t[:]),
                 [xn_t.b, ident.b], [bk.b], inc=(kc == 7))
        yield
        k.op(act, lambda: S_.copy(out=xnT_t.t[:].rearrange("p a b -> p (a b)"), in_=pv), [bk.b], [xnT_t.b])
        yield

    def stage_A(i):
        X = Xs[i % NX]
        s2 = i % 2
        k.dma(sp, lambda: nc.sync.dma_start(out=X.t[:], in_=x_d[i * 128:(i + 1) * 128, :]), reads=[x_db], writes=[X.b])
        col = ncol[i % 8]
        xT = xnTA[0]
        yield from norm_T("A", X, xnA[0], xT, col)
        ug_, vg_, vn_, fS_, lf_, vb_, gs_, sq_ = ug[s2], vg[0], vn[s2], fS[0], logf[0], vb[s2], gs[s2], sq[0]

        def tok_group(c):
            bk = psS("A")
            mm_group(bk, bk.t[:, :], [(xT.t[:, kc, :], w_in.t[:, kc, c * 512:(c + 1) * 512]) for kc in range(8)], [xT.b, w_in.b])
            return bk
        b_f = tok_group(3)
        yield
        b_i = tok_group(4)
        k.op(act, lambda: S_.activation(out=fS_.t[:], in_=b_f.t[:, :], func=AF.Sigmoid), [b_f.b], [fS_.b])
        yield
        b_v = tok_group(1)
        k.op(dve, lambda: V_.tensor_copy(out=vb_.t[:], in_=b_i.t[:, :]), [b_i.b], [vb_.b])
        k.op(pool, lambda: G_.tensor_tensor(out=fS_.t[:], in0=fS_.t[:], in1=omlb_b.t[:], op=ALU.mult), [fS_.b, omlb_b.b], [fS_.b])
        k.op(pool, lambda: G_.tensor_tensor(out=fS_.t[:], in0=fS_.t[:], in1=lb_b.t[:], op=ALU.add), [fS_.b, lb_b.b], [fS_.b])
        yield
        b_u = tok_group(0)
        k.op(act, lambda: S_.activation(out=vg_.t[:], in_=b_v.t[:, :], func=AF.Gelu_apprx_tanh), [b_v.b], [vg_.b])
        yield
        b_g = tok_group(5)
        k.op(act, lambda: S_.activation(out=ug_.t[:], in_=b_u.t[:, :], func=AF.Gelu_apprx_tanh), [b_u.b], [ug_.b])
        k.op(act, lambda: S_.activation(out=lf_.t[:], in_=fS_.t[:], func=AF.Ln), [fS_.b], [lf_.b])
        st_ = vst[s2]
        k.op(dve, lambda: V_.bn_stats(out=st_.t[:, 0:6], in_=vg_.t[:]), [vg_.b], [st_.b])
        k.op(dve, lambda: V_.bn_aggr(out=col.t[:, 2:4], in_=st_.t[:, 0:6]), [st_.b], [col.b])
        rsqrt_col(col.t[:, 3:4], col.b, col.t[:, 3:4], col.b, 1.0)
        yield
        b_q = psS("A")
        for h in range(4):
            mm_group(b_q, b_q.t[:, h * 128:(h + 1) * 128],
                     [(w_in.t[:, kc, 1024 + h * 128:1024 + (h + 1) * 128], xT.t[:, kc, :]) for kc in range(8)], [xT.b, w_in.b])
        k.op(act, lambda: S_.activation(out=gs_.t[:], in_=b_g.t[:, :], func=AF.Silu), [b_g.b], [gs_.b])
        k.op(dve, lambda: V_.scalar_tensor_tensor(out=col.t[:, 4:5], in0=col.t[:, 2:3], scalar=-1.0, in1=col.t[:, 3:4], op0=ALU.mult, op1=ALU.mult),
             [col.b], [col.b])
        yield
        b_b = psS("A")
        k.op(pe, lambda: TE.matmul(b_b.t[:, :], lhsT=mext[:, 0:128], rhs=lf_.t[:, :], start=True, stop=True), [cst.b, lf_.b], [b_b.b])
        k.op(act, lambda: S_.activation(out=sq_.t[:], in_=b_q.t[:, :], func=AF.Silu), [b_q.b], [sq_.b])
        k.op(act, lambda: S_.activation(out=vg_.t[:], in_=vg_.t[:], func=AF.Identity, scale=col.t[:, 3:4], bias=col.t[:, 4:5]),
             [vg_.b, col.b], [vg_.b])
        yield
        b_bT = psS("A")
        for h in range(4):
            k.op(pe, lambda h=h: TE.matmul(b_bT.t[:, h * 128:(h + 1) * 128], lhsT=lf_.t[:, h * 128:(h + 1) * 128], rhs=mext[:, 0:128], start=True, stop=True),
                 [cst.b, lf_.b], [b_bT.b], inc=(h == 3))
        k.op(dve, lambda: V_.tensor_tensor(out=vn_.t[:], in0=vg_.t[:], in1=lng_b.t[:], op=ALU.mult), [vg_.b, lng_b.b], [vn_.b])
        yield
        b_cd = psS("A")
        for h in range(4):
            k.op(pe, lambda h=h: TE.matmul(b_cd.t[:, h * 2:(h + 1) * 2], lhsT=lf_.t[:, h * 128:(h + 1) * 128], rhs=mext[:, 128:130], start=True, stop=True),
                 [cst.b, lf_.b], [b_cd.b], inc=(h == 3))
        en_, ki_, kT_, eb_, cd_, qd_ = logf[0], kinv[s2], kinvT[s2], ebT[0], cde[s2], qdT[s2]
        k.op(act, lambda: S_.activation(out=eb_.t[:], in_=b_bT.t[:, :], func=AF.Exp), [b_bT.b], [eb_.b])
        yield
        k.op(act, lambda: S_.activation(out=cd_.t[:, 0:8], in_=b_cd.t[:, 0:8], func=AF.Exp), [b_cd.b], [cd_.b])
        k.op(act, lambda: S_.activation(out=en_.t[:], in_=b_b.t[:, :], func=AF.Exp, scale=-1.0), [b_b.b], [en_.b])
        k.op(dve, lambda: V_.tensor_copy(out=cd_.t[:, 8:12], in_=eb_.t[:].rearrange("p (h t) -> p h t", h=4)[:, :, 127]), [eb_.b], [cd_.b])
        k.op(dve, lambda: V_.tensor_tensor(out=qd_.t[:], in0=sq_.t[:], in1=eb_.t[:], op=ALU.mult), [sq_.b, eb_.b], [qd_.b])
        yield
        k.op(dve, lambda: V_.scalar_tensor_tensor(out=ki_.t[:], in0=fS_.t[:], scalar=1.0, in1=en_.t[:], op0=ALU.subtract, op1=ALU.mult),
             [fS_.b, en_.b], [ki_.b])
        yield
        bk = psS("A")
        pv = bk.t[:].bitcast(BF16)
        for h in range(4):
            k.op(pe, lambda h=h: TE.transpose(out=pv[:, h * 128:(h + 1) * 128], in_=ki_.t[:, h * 128:(h + 1) * 128], identity=ident.t[:]),
                 [ki_.b, ident.b], [bk.b], inc=(h == 3))
        yield
        k.op(act, lambda: S_.copy(out=kT_.t[:], in_=pv[:, 0:512]), [bk.b], [kT_.b])

    def stage_B(i):
        s2 = i % 2
        col = ncol[i % 8]
        ug_, vn_, vb_, gs_ = ug[s2], vn[s2], vb[s2], gs[s2]
        ki_, kT_, cd_, qd_ = kinv[s2], kinvT[s2], cde[s2], qdT[s2]
        sc_, Sp_, yc_, yy_, ut_ = scT[s2], Sp[s2], ycat[s2], yy[0], yy[0]
        if i % NT == 0 and i > 0:
            k.op(pool, lambda: G_.memset(Sst.t[:], 0.0), [], [Sst.b])
        b_s = psS("B")
        for h in range(4):
            k.op(pe, lambda h=h: TE.matmul(b_s.t[:, h * 128:(h + 1) * 128], lhsT=kT_.t[:, h * 128:(h + 1) * 128], rhs=qd_.t[:, h * 128:(h + 1) * 128], start=True, stop=True),
                 [kT_.b, qd_.b], [b_s.b], inc=(h == 3))
        b_z = psS("B")
        for g in range(4):
            k.op(pe, lambda g=g: TE.matmul(b_z.t[:, g * 128:(g + 1) * 128], lhsT=wsT.t[:, g, :], rhs=vn_.t[:, g * 128:(g + 1) * 128], start=True, stop=True),
                 [wsT.b, vn_.b], [b_z.b], inc=(g == 3))
        v4 = lambda ap: ap.rearrange("p (h v) -> p h v", h=4)
        cdv = cd_.t[:, 0:8].rearrange("p (h two) -> p h two", two=2)
        k.op(dve, lambda: V_.tensor_tensor(out=v4(Sp_.t[:]), in0=v4(Sst.t[:]), in1=cdv[:, :, 0:1].to_broadcast([128, 4, 128]), op=ALU.mult),
             [Sst.b, cd_.b], [Sp_.b])
        yield
        k.op(dve, lambda: V_.tensor_tensor(out=sc_.t[:], in0=b_s.t[:, :], in1=negm4.t[:].rearrange("p a b -> p (a b)"), op=ALU.mult),
             [b_s.b, negm4.b], [sc_.b])
        for g in range(4):
            k.op(dve, lambda g=g: V_.scalar_tensor_tensor(out=yy_.t[:, g * 128:(g + 1) * 128], in0=b_z.t[:, g * 128:(g + 1) * 128], scalar=bspT.t[:, g:g + 1],
                                                          in1=ug_.t[:, g * 128:(g + 1) * 128], op0=ALU.add, op1=ALU.mult),
                 [b_z.b, bspT.b, ug_.b], [yy_.b])
        yield
        b_o = psS("B")
        for h in range(4):
            hs_ = slice(h * 128, (h + 1) * 128)
            k.op(pe, lambda hs_=hs_: TE.matmul(b_o.t[:, hs_], lhsT=sc_.t[:, hs_], rhs=vb_.t[:, hs_], start=True, stop=False),
                 [sc_.b, vb_.b], [b_o.b], inc=False)
            k.op(pe, lambda hs_=hs_: TE.matmul(b_o.t[:, hs_], lhsT=qd_.t[:, hs_], rhs=Sp_.t[:, hs_], start=False, stop=True),
                 [qd_.b, Sp_.b], [b_o.b], inc=(h == 3))
        b_U = psS("B")
        for h in range(4):
            hs_ = slice(h * 128, (h + 1) * 128)
            k.op(pe, lambda hs_=hs_: TE.matmul(b_U.t[:, hs_], lhsT=ki_.t[:, hs_], rhs=vb_.t[:, hs_], start=True, stop=True),
                 [ki_.b, vb_.b], [b_U.b], inc=(h == 3))
        sumsq(yy_.t[:], yy_.b, yc_, col.t[:, 5:6], col.b, n=512)
        rsqrt_col(col.t[:, 5:6], col.b, col.t[:, 5:6], col.b, 1.0 / 512)
        yield
        k.op(dve, lambda: V_.tensor_scalar(out=yc_.t[:, 0:512], in0=yy_.t[:], scalar1=col.t[:, 5:6], scalar2=None, op0=ALU.mult),
             [yy_.b, col.b], [yc_.b])
        for h in range(4):
            hs_ = slice(h * 128, (h + 1) * 128)
            k.op(act, lambda hs_=hs_, h=h: S_.activation(out=yc_.t[:, 512 + h * 128:512 + (h + 1) * 128], in_=b_o.t[:, hs_], func=AF.Square, accum_out=col.t[:, 8 + h:9 + h]),
                 [b_o.b], [yc_.b, col.b])
        rsqrt_col(col.t[:, 8:12], col.b, col.t[:, 8:12], col.b, 1.0 / 128, ncol=4)
        yield
        k.op(dve, lambda: V_.tensor_tensor(out=v4(yy_.t[:]), in0=v4(b_o.t[:, :]), in1=col.t[:, 8:12].unsqueeze(2).to_broadcast([128, 4, 128]), op=ALU.mult),
             [b_o.b, col.b], [yy_.b])
        k.op(dve, lambda: V_.tensor_tensor(out=yc_.t[:, 512:1024], in0=yy_.t[:], in1=gs_.t[:], op=ALU.mult), [yy_.b, gs_.b], [yc_.b])
        k.op(dve, lambda: V_.tensor_tensor(out=v4(ut_.t[:]), in0=v4(b_U.t[:, :]), in1=cd_.t[:, 8:12].unsqueeze(2).to_broadcast([128, 4, 128]), op=ALU.mult),
             [b_U.b, cd_.b], [ut_.b])
        k.op(dve, lambda: V_.tensor_tensor(out=v4(Sst.t[:]), in0=v4(Sst.t[:]), in1=cdv[:, :, 1:2].to_broadcast([128, 4, 128]), op=ALU.mult),
             [Sst.b, cd_.b], [Sst.b])
        k.op(dve, lambda: V_.tensor_tensor(out=Sst.t[:], in0=Sst.t[:], in1=ut_.t[:], op=ALU.subtract), [Sst.b, ut_.b], [Sst.b])

    def stage_C(i):
        X = Xs[i % NX]
        s2 = i % 2
        col = ncol[i % 8]
        yc_, ycT_ = ycat[s2], ycatT[0]
        bk = psS("C1")
        pv = bk.t[:].bitcast(BF16)
        for kc in range(8):
            k.op(pe, lambda kc=kc: TE.transpose(out=pv[:, kc * 128:(kc + 1) * 128], in_=yc_.t[:, kc * 128:(kc + 1) * 128], identity=ident.t[:]),
                 [yc_.b, ident.b], [bk.b], inc=(kc == 7))
        yield
        k.op(act, lambda: S_.copy(out=ycT_.t[:].rearrange("p a b -> p (a b)"), in_=pv), [bk.b], [ycT_.b])
        yield
        for hf in range(2):
            bk = psS("C1")
            mm_group(bk, bk.t[:, :], [(ycT_.t[:, kc, :], w_out.t[:, kc, hf * 512:(hf + 1) * 512]) for kc in range(8)], [ycT_.b, w_out.b])
            yield
            k.op(dve, lambda hf=hf, bk=bk: V_.tensor_tensor(out=X.t[:, hf * 512:(hf + 1) * 512], in0=X.t[:, hf * 512:(hf + 1) * 512], in1=bk.t[:, :], op=ALU.add),
                 [X.b, bk.b], [X.b])
        dbg_dump(X.t[:], X.b, dbg_h1 if dbg else None, i)
        xT = xnTC[0]
        yield from norm_T("C1", X, xnC[0], xT, col)
        qT_ = qTa[s2]
        for half in range(2):
            bk = psS("C1")
            for cc in range(4):
                c = half * 4 + cc
                mm_group(bk, bk.t[:, cc * 128:(cc + 1) * 128], [(w_xq.t[:, kc, c * 128:(c + 1) * 128], xT.t[:, kc, :]) for kc in range(8)], [xT.b, w_xq.b])
            yield
            if half == 0:
                k.op(act, lambda bk=bk: S_.copy(out=qT_.t[:, 0:4, :].rearrange("p a b -> p (a b)"), in_=bk.t[:, :]), [bk.b], [qT_.b])
            else:
                k.op(dve, lambda bk=bk: V_.tensor_copy(out=qT_.t[:, 4:8, :].rearrange("p a b -> p (a b)"), in_=bk.t[:, :]), [bk.b], [qT_.b])

    def stage_C2(i):
        X = Xs[i % NX]
        s2 = i % 2
        kv_ = kvr[0]
        qT_ = qTa[s2]
        if i % NT == 0:
            s = i // NT
            k.dma(sp, lambda: nc.sync.dma_start(out=kv_.t[:], in_=kv_d[s, :, :]), reads=[kv_db[s]], writes=[kv_.b])
        ET_ = ET[0]
        for mc in range(2):
            bk = psS("C2")
            for h in range(4):
                mm_group(bk, bk.t[:, h * 128:(h + 1) * 128],
                         [(kv_.t[:, (2 * h + dc) * 256 + mc * 128:(2 * h + dc) * 256 + (mc + 1) * 128], qT_.t[:, 2 * h + dc, :]) for dc in range(2)], [kv_.b, qT_.b])
            yield
            k.op(act, lambda bk=bk, mc=mc: S_.activation(out=ET_.t[:, mc, :, :].rearrange("p a b -> p (a b)"), in_=bk.t[:, :], func=AF.Exp, scale=1.0 / 16.0),
                 [bk.b], [ET_.b])
        yield
        b_r = psS("C2")
        mm_group(b_r, b_r.t[:, :], [(ones_bf.t[:], ET_.t[:, mc, :, :].rearrange("p a b -> p (a b)")) for mc in range(2)], [ones_bf.b, ET_.b])
        yield
        Ri = Rinv[0]
        k.op(dve, lambda: V_.reciprocal(out=Ri.t[:].rearrange("p a b -> p (a b)"), in_=b_r.t[:, :]), [b_r.b], [Ri.b])
        oT_ = oT[0]
        for half in range(2):
            bk = psS("C2")
            for cc in range(4):
                c = half * 4 + cc
                h, dc = c // 2, c % 2
                mm_group(bk, bk.t[:, cc * 128:(cc + 1) * 128],
                         [(kv_.t[:, 2048 + mc * 1024 + h * 256 + dc * 128:2048 + mc * 1024 + h * 256 + (dc + 1) * 128], ET_.t[:, mc, h, :]) for mc in range(2)], [kv_.b, ET_.b])
            yield
            k.op(dve, lambda bk=bk, half=half: V_.tensor_tensor(out=oT_.t[:, 4 * half:4 * half + 4, :].rearrange("p (h c) t -> p h c t", h=2),
                                                                in0=bk.t[:, :].rearrange("p (h c t) -> p h c t", h=2, c=2),
                                                                in1=Ri.t[:, 2 * half:2 * half + 2, :].unsqueeze(2).to_broadcast([128, 2, 2, 128]), op=ALU.mult),
                 [bk.b, Ri.b], [oT_.b])
        yield
        for hf in range(2):
            bk = psS("C2")
            mm_group(bk, bk.t[:, :], [(oT_.t[:, c, :], w_xo.t[:, c, hf * 512:(hf + 1) * 512]) for c in range(8)], [oT_.b, w_xo.b])
            yield
            k.op(dve, lambda hf=hf, bk=bk: V_.tensor_tensor(out=X.t[:, hf * 512:(hf + 1) * 512], in0=X.t[:, hf * 512:(hf + 1) * 512], in1=bk.t[:, :], op=ALU.add),
                 [X.b, bk.b], [X.b])
        dbg_dump(X.t[:], X.b, dbg_h2 if dbg else None, i)

    def stage_D(i):
        X = Xs[i % NX]
        s2 = i % 2
        col = ncol[i % 8]
        xT, xn_ = xnTD[0], xnD[0]
        k.dma(sp, lambda: nc.sync.dma_start(out=hs_d[i * 128:(i + 1) * 128, :], in_=X.t[:]), reads=[X.b], writes=[hs_db[i]])
        yield from norm_T("D", X, xn_, xT, col, gain=gffn_b)
        b_l = psS("D")
        mm_group(b_l, b_l.t[:, 0:68], [(xT.t[:, kc, :], w_r.t[:, kc, :]) for kc in range(8)], [xT.b, w_r.b])
        yield
        lg_, r_, Mb_ = lg[s2], rt[s2], Mb[s2]
        R = r_.t
        k.op(dve, lambda: V_.tensor_tensor(out=lg_.t[:], in0=b_l.t[:, 0:68], in1=br_b.t[:], op=ALU.add), [b_l.b, br_b.b], [lg_.b])
        k.op(dve, lambda: V_.tensor_reduce(out=R[:, 0:1], in_=lg_.t[:, 64:68], axis=AX.X, op=ALU.max), [lg_.b], [r_.b])
        yield
        k.op(pool, lambda: G_.tensor_scalar(out=R[:, 1:2], in0=R[:, 0:1], scalar1=-1.0, scalar2=None, op0=ALU.mult), [r_.b], [r_.b])
        k.op(pool, lambda: G_.tensor_scalar(out=R[:, 32:64], in0=lg_.t[:, 32:64], scalar1=R[:, 1:2], scalar2=1e30, op0=ALU.add, op1=ALU.mult), [lg_.b, r_.b], [r_.b])
        k.op(pool, lambda: G_.tensor_tensor(out=R[:, 32:64], in0=R[:, 32:64], in1=lg_.t[:, 0:32], op=ALU.add), [r_.b, lg_.b], [r_.b])
        yield
        k.op(dve, lambda: V_.max(out=R[:, 4:12], in_=R[:, 32:64]), [r_.b], [r_.b])
        yield
        k.op(pool, lambda: G_.tensor_scalar(out=R[:, 64:96], in0=R[:, 32:64], scalar1=R[:, 4:5], scalar2=None, op0=ALU.is_equal), [r_.b], [r_.b])
        k.op(pool, lambda: G_.tensor_scalar(out=R[:, 96:128], in0=R[:, 32:64], scalar1=R[:, 5:6], scalar2=None, op0=ALU.is_equal), [r_.b], [r_.b])
        k.op(pool, lambda: G_.tensor_tensor(out=Mb_.t[:], in0=R[:, 64:96], in1=R[:, 96:128], op=ALU.add), [r_.b], [Mb_.b])
        k.op(pool, lambda: G_.tensor_tensor(out=R[:, 12:13], in0=R[:, 5:6], in1=R[:, 4:5], op=ALU.subtract), [r_.b], [r_.b])
        yield
        b_p = psS("D")
        k.op(pe, lambda: TE.matmul(b_p.t[:, 0:32], lhsT=slt_bf.t[:], rhs=Mb_.t[:], start=True, stop=False), [slt_bf.b, Mb_.b], [b_p.b], inc=False)
        k.op(pe, lambda: TE.matmul(b_p.t[:, 0:32], lhsT=ones_bf.t[:], rhs=Macc.t[:], start=False, stop=True), [ones_bf.b, Macc.b], [b_p.b])
        k.op(act, lambda: S_.activation(out=R[:, 20:24], in_=lg_.t[:, 64:68], func=AF.Exp, bias=R[:, 1:2], accum_out=R[:, 2:3]), [lg_.b, r_.b], [r_.b])
        k.op(act, lambda: S_.activation(out=R[:, 13:14], in_=R[:, 12:13], func=AF.Exp), [r_.b], [r_.b])
        yield
        k.op(dve, lambda: V_.scalar_tensor_tensor(out=R[:, 128:160], in0=b_p.t[:, 0:32], scalar=float(CAP - 1), in1=ecap, op0=ALU.min, op1=ALU.add), [b_p.b, cst.b], [r_.b])
        k.op(pool, lambda: G_.tensor_tensor(out=Macc.t[:], in0=Macc.t[:], in1=Mb_.t[:], op=ALU.add), [Macc.b, Mb_.b], [Macc.b])
        yield
        k.op(pool, lambda: G_.tensor_tensor(out=R[:, 64:96], in0=R[:, 64:96], in1=R[:, 128:160], op=ALU.mult), [r_.b], [r_.b])
        k.op(pool, lambda: G_.tensor_tensor(out=R[:, 96:128], in0=R[:, 96:128], in1=R[:, 128:160], op=ALU.mult), [r_.b], [r_.b])
        k.op(pool, lambda: G_.tensor_scalar(out=R[:, 14:15], in0=R[:, 13:14], scalar1=1.0, scalar2=R[:, 2:3], op0=ALU.add, op1=ALU.mult), [r_.b], [r_.b])
        k.op(pool, lambda: G_.tensor_tensor(out=gates.t[:, i, 0:1], in0=R[:, 14:15], in1=nhalf.t[:, 7:8], op=ALU.pow), [r_.b, nhalf.b], [gates_b[i]])
        k.op(pool, lambda: G_.tensor_tensor(out=gates.t[:, i, 1:2], in0=R[:, 13:14], in1=gates.t[:, i, 0:1], op=ALU.mult), [r_.b, gates_b[i]], [gates_b[i]])
        yield
        k.op(dve, lambda: V_.tensor_reduce(out=R[:, 16:18], in_=R[:, 64:128].rearrange("p (a b) -> p a b", a=2), axis=AX.X, op=ALU.add), [r_.b], [r_.b])
        k.op(dve, lambda: V_.tensor_scalar(out=dest.t[:, i, :], in0=R[:, 16:18], scalar1=float(NSLOT - 1), scalar2=None, op0=ALU.min), [r_.b], [dest_b[i]])
        yield
        for kk in range(2):
            k.dma(pool, lambda kk=kk: G_.indirect_dma_start(out=xs_d[:, :], out_offset=bass.IndirectOffsetOnAxis(ap=dest.t[:, i, kk:kk + 1], axis=0),
                                                           in_=xn_.t[:], in_offset=None),
                  reads=[xn_.b, dest_b[i]], writes=[xs_db[2 * i + kk]])

    stages = [stage_A, stage_B, stage_C, stage_C2, stage_D]
    nst = len(stages)
    prereq = {
        0: lambda i: [(0, i - 1), (1, i - 2), (4, i - 5)],
        1: lambda i: [(0, i), (1, i - 1), (2, i - 2)],
        2: lambda i: [(1, i), (2, i - 1), (3, i - 2)],
        3: lambda i: [(2, i), (3, i - 1)],
        4: lambda i: [(3, i), (4, i - 1)],
    }
    done = set()
    nxt = [0] * nst
    active = []
    while len(done) < nst * TT:
        for s_i in reversed(range(nst)):
            i_ = nxt[s_i]
            if i_ < TT and all((p[1] < 0) or (p in done) for p in prereq[s_i](i_)):
                active.append(((s_i, i_), stages[s_i](i_)))
                nxt[s_i] += 1
        prio_ = {4: 0, 0: 1, 1: 2, 2: 3, 3: 4}
        for item in sorted(active, key=lambda it: prio_[it[0][0]]):
            try:
                next(item[1])
            except StopIteration:
                active.remove(item)
                done.add(item[0])
    k.barrier()
    pop_scope()

    push_scope()
    wg = sbn("wg", [128, 8, 512], BF16, 2)
    wu = sbn("wu", [128, 8, 512], BF16, 2)
    wd = sbn("wd", [128, 4, 1024], BF16, 2)
    xsT = sbn("xsT", [128, 8, CAP], BF16, 2)
    hid = sbn("hid", [128, 4, CAP], BF16, 2)
    sgt = sbn("sgt", [128, 512], F32, 2)
    yo = sbn("yo", [128, D], BF16, 3)
    gfin_b = sb("gfin_b", [128, D], F32)
    hsT = sbn("hsT", [128, D], F32, 3)
    y0 = sbn("y0", [128, D], BF16, 3)
    fo = sbn("fo", [128, D], F32, 2)
    y1 = sbn("y1", [128, D], BF16, 3)
    fcol = sbn("fcol", [128, 2], F32, 3)
    epsc = sb("epsc", [128, 1], F32)
    k.op(pool, lambda: G_.memset(epsc.t[:], EPS), [], [epsc.b])
    fjunk = sb("fjunk", [128, D], BF16)
    k.dma(sp, lambda: nc.sync.dma_start(out=gfin_b.t[:], in_=gfin_d[0:1, :].partition_broadcast(128)), writes=[gfin_b.b])
    wdb = Buf("wdram")
    yb_db = Buf("yb")
    blocks = []
    o_ = 0
    while o_ < CAP:
        w_ = min(512, CAP - o_)
        blocks.append((o_, w_))
        o_ += w_
    yo_i = 0
    sg_i = 0
    NCH = CAP // 128
    xtok = sbn("xtok", [128, D], BF16, 3)
    xtok_i = [0]

    def exp_loads(e):
        se = e % 2
        wg_, wu_, wd_ = wg[se], wu[se], wd[se]
        k.dma(pool, lambda: G_.dma_start(out=wg_.t[:], in_=w_eg_d[e].rearrange("(kc p) f -> p kc f", p=128)), reads=[wdb], writes=[wg_.b])
        k.dma(pool, lambda: G_.dma_start(out=wu_.t[:], in_=w_eu_d[e].rearrange("(kc p) f -> p kc f", p=128)), reads=[wdb], writes=[wu_.b])
        k.dma(pool, lambda: G_.dma_start(out=wd_.t[:], in_=w_ed_d[e].rearrange("(fc p) d -> p fc d", p=128)), reads=[wdb], writes=[wd_.b])

    def exp_xpose(e, sc):
        xT_ = xsT[e % 2]
        xt_ = xtok[xtok_i[0] % 3]
        xtok_i[0] += 1
        r0 = e * CAP + sc * 128
        k.dma(sp, lambda: nc.sync.dma_start(out=xt_.t[:], in_=xs_d[r0:r0 + 128, :]), reads=[wdb], writes=[xt_.b])
        bk = ps()
        pv = bk.t[:].bitcast(BF16)
        for kc in range(8):
            k.op(pe, lambda kc=kc: TE.transpose(out=pv[:, kc * 128:(kc + 1) * 128], in_=xt_.t[:, kc * 128:(kc + 1) * 128], identity=ident.t[:]),
                 [xt_.b, ident.b], [bk.b], inc=(kc == 7))
        if sc % 2 == 0:
            k.op(act, lambda: S_.copy(out=xT_.t[:, :, sc * 128:(sc + 1) * 128], in_=pv.rearrange("p (a b) -> p a b", a=8)), [bk.b], [xT_.b])
        else:
            k.op(dve, lambda: V_.tensor_copy(out=xT_.t[:, :, sc * 128:(sc + 1) * 128], in_=pv.rearrange("p (a b) -> p a b", a=8)), [bk.b], [xT_.b])

    def exp_compute(e):
        nonlocal yo_i, sg_i
        se = e % 2
        wg_, wu_, wd_, xT_, hd_ = wg[se], wu[se], wd[se], xsT[se], hid[se]
        nxt_sc = 0
        for (o0, w0) in blocks:
            for fc in range(4):
                b_g, b_u = ps(), ps()
                mm_group(b_g, b_g.t[:, 0:w0], [(wg_.t[:, kc, fc * 128:(fc + 1) * 128], xT_.t[:, kc, o0:o0 + w0]) for kc in range(8)], [wg_.b, xT_.b])
                mm_group(b_u, b_u.t[:, 0:w0], [(wu_.t[:, kc, fc * 128:(fc + 1) * 128], xT_.t[:, kc, o0:o0 + w0]) for kc in range(8)], [wu_.b, xT_.b])
                sg_ = sgt[sg_i % 2]
                sg_i += 1
                k.op(act, lambda: S_.activation(out=sg_.t[:, 0:w0], in_=b_g.t[:, 0:w0], func=AF.Silu), [b_g.b], [sg_.b])
                k.op(dve, lambda: V_.tensor_tensor(out=hd_.t[:, fc, o0:o0 + w0], in0=sg_.t[:, 0:w0], in1=b_u.t[:, 0:w0], op=ALU.mult), [sg_.b, b_u.b], [hd_.b])
                if e + 1 < NEXP and nxt_sc < NCH:
                    exp_xpose(e + 1, nxt_sc)
                    nxt_sc += 1
        for sc in range(NCH):
            yo_ = yo[yo_i % 3]
            yo_i += 1
            for hf in range(2):
                bk = ps()
                mm_group(bk, bk.t[:, :], [(hd_.t[:, fc, sc * 128:(sc + 1) * 128], wd_.t[:, fc, hf * 512:(hf + 1) * 512]) for fc in range(4)], [hd_.b, wd_.b])
                if hf == 0:
                    k.op(act, lambda: S_.copy(out=yo_.t[:, 0:512], in_=bk.t[:, :]), [bk.b], [yo_.b])
                else:
                    k.op(dve, lambda: V_.tensor_copy(out=yo_.t[:, 512:1024], in_=bk.t[:, :]), [bk.b], [yo_.b])
            r0 = e * CAP + sc * 128
            k.dma(act, lambda: S_.dma_start(out=yb_d[r0:r0 + 128, :], in_=yo_.t[:]), reads=[yo_.b], writes=[Buf()])
        while e + 1 < NEXP and nxt_sc < NCH:
            exp_xpose(e + 1, nxt_sc)
            nxt_sc += 1

    exp_loads(0)
    for sc in range(NCH):
        exp_xpose(0, sc)
    for e in range(NEXP):
        if e + 1 < NEXP:
            exp_loads(e + 1)
        exp_compute(e)
    k.barrier()

    out_db = Buf("out")
    NF = 3

    def fin_loads(i):
        s3 = i % NF
        h_, a_, b_ = hsT[s3], y0[s3], y1[s3]
        k.dma(sp, lambda: nc.sync.dma_start(out=h_.t[:], in_=hs_d[i * 128:(i + 1) * 128, :]), reads=[wdb], writes=[h_.b])
        k.dma(pool, lambda: G_.indirect_dma_start(out=a_.t[:], out_offset=None, in_=yb_d[:, :], in_offset=bass.IndirectOffsetOnAxis(ap=dest.t[:, i, 0:1], axis=0)),
              reads=[wdb, dest_b[i]], writes=[a_.b])
        k.dma(pool, lambda: G_.indirect_dma_start(out=b_.t[:], out_offset=None, in_=yb_d[:, :], in_offset=bass.IndirectOffsetOnAxis(ap=dest.t[:, i, 1:2], axis=0)),
              reads=[wdb, dest_b[i]], writes=[b_.b])

    def fin_compute(i):
        s3 = i % NF
        h_, a_, b_, c_ = hsT[s3], y0[s3], y1[s3], fcol[s3]
        k.op(dve, lambda: V_.scalar_tensor_tensor(out=h_.t[:], in0=a_.t[:], scalar=gates.t[:, i, 0:1], in1=h_.t[:], op0=ALU.mult, op1=ALU.add),
             [a_.b, gates_b[i], h_.b], [h_.b])
        k.op(dve, lambda: V_.scalar_tensor_tensor(out=h_.t[:], in0=b_.t[:], scalar=gates.t[:, i, 1:2], in1=h_.t[:], op0=ALU.mult, op1=ALU.add),
             [b_.b, gates_b[i], h_.b], [h_.b])
        k.op(act, lambda: S_.activation(out=fjunk.t[:], in_=h_.t[:], func=AF.Square, accum_out=c_.t[:, 0:1]), [h_.b], [fjunk.b, c_.b])
        k.op(act, lambda: S_.activation(out=c_.t[:, 0:1], in_=c_.t[:, 0:1], func=AF.Sqrt, scale=1.0 / D, bias=epsc.t[:, 0:1]), [c_.b, epsc.b], [c_.b])
        k.op(dve, lambda: V_.reciprocal(out=c_.t[:, 0:1], in_=c_.t[:, 0:1]), [c_.b], [c_.b])
        fo_ = fo[i % 2]
        k.op(dve, lambda: V_.scalar_tensor_tensor(out=fo_.t[:], in0=h_.t[:], scalar=c_.t[:, 0:1], in1=gfin_b.t[:], op0=ALU.mult, op1=ALU.mult),
             [h_.b, c_.b, gfin_b.b], [fo_.b])
        k.dma(act, lambda: S_.dma_start(out=out_d[i * 128:(i + 1) * 128, :], in_=fo_.t[:]), reads=[fo_.b], writes=[out_db])

    fin_loads(0)
    if TT > 1:
        fin_loads(1)
    for i in range(TT):
        if i + 2 < TT:
            fin_loads(i + 2)
        fin_compute(i)
    k.barrier()
    pop_scope()
    pop_scope()
    k.close()
    return nc


def _consts(CAP):
    c = np.zeros((128, 674), np.float32)
    s = np.arange(128)[:, None]
    t = np.arange(128)[None, :]
    c[:, 0:128] = (s == t)
    c[:, 128:256] = (s <= t)
    c[:, 256:384] = (s < t)
    c[:, 384:512] = 1.0
    m = np.zeros((128, 130), np.float32)
    m[:, 0:128] = ((s >= 64) & (s <= t)).astype(np.float32) - ((s <= 63) & (s > t)).astype(np.float32)
    m[:, 128] = (np.arange(128) <= 63)
    m[:, 129] = 1.0
    c[:, 512:642] = m
    c[:, 642:674] = (np.arange(32) * CAP)[None, :]
    return c


def _prep_shared(inp, CAP):
    f = lambda a: np.ascontiguousarray(np.asarray(a, dtype=np.float32))
    col = lambda v: f(v).reshape(8, 128).T
    w_rg = f(inp["w_router_group"][0])
    w_re = f(inp["w_router_expert"][0])
    b_rg = f(inp["b_router_group"][0])
    b_re = f(inp["b_router_expert"][0])
    w_r = np.concatenate([w_re, np.repeat(w_rg, 8, axis=1), w_rg], axis=1)
    b_r = np.concatenate([b_re, np.repeat(b_rg, 8), b_rg])[None, :]
    gcols = np.concatenate([col(inp["norm_mix"][0]), col(inp["norm_xattn"][0]), col(inp["norm_mem"][0]),
                            col(np.concatenate([f(inp["gmlp_beta"][0]), f(inp["hgrn_out_gain"][0])]))], axis=1)
    return {
        "w_in": f(inp["w_in"][0]), "w_out": f(inp["w_out"][0]), "w_xq": f(inp["w_xq"][0]),
        "w_xkv": f(inp["w_xkv"][0]), "w_xo": f(inp["w_xo"][0]),
        "w_eg": f(inp["w_expert_gate"][0]), "w_eu": f(inp["w_expert_up"][0]), "w_ed": f(inp["w_expert_down"][0]),
        "w_r": f(w_r), "b_r": f(b_r), "gcols": f(gcols),
        "lng": f(inp["gmlp_ln"][0])[None, :], "lbl": f(inp["hgrn_lb_logits"]),
        "gffn": f(inp["norm_ffn"][0])[None, :], "gfin": f(inp["norm_final"])[None, :],
        "wspT": f(np.transpose(f(inp["gmlp_w_spatial"][0]), (2, 0, 1))),
        "bspT": f(f(inp["gmlp_b_spatial"][0]).T),
        "cst": _consts(CAP),
    }


CAP_FULL = 1152


def run(inp, n_cores, NSEQ, S, CAP, dbg=False):
    nc = build_nc(NSEQ, S, CAP, dbg=dbg)
    shared = _prep_shared(inp, CAP)
    x = np.asarray(inp["x"], dtype=np.float32)
    mem = np.asarray(inp["mem"], dtype=np.float32)
    in_maps = []
    for c in range(n_cores):
        m = dict(shared)
        m["x"] = np.ascontiguousarray(x[c * NSEQ:(c + 1) * NSEQ].reshape(NSEQ * S, D))
        m["mem"] = np.ascontiguousarray(mem[c * NSEQ:(c + 1) * NSEQ].reshape(NSEQ * 256, D))
        in_maps.append(m)
    res = run_bass_kernel_spmd(nc, in_maps, core_ids=list(range(n_cores)))
    return res


def kernel(**inputs):
    B, S, _ = inputs["x"].shape
    NSEQ = B // N_CORES
    res = run(inputs, N_CORES, NSEQ, S, CAP_FULL)
    out = np.concatenate([r["out"].reshape(NSEQ, S, D) for r in res.results], axis=0)
    return out.astype(np.float32)
```

```python
import numpy as np
import concourse.bass as bass
import concourse.mybir as mybir
from concourse.bass_utils import run_bass_kernel_spmd

F32 = mybir.dt.float32
BF16 = mybir.dt.bfloat16
I32 = mybir.dt.int32
AF = mybir.ActivationFunctionType
ALU = mybir.AluOpType
AX = mybir.AxisListType

N_CORES = 8
D = 1024
EPS = 1e-6
NEXP = 32


class Buf:
    __slots__ = ("name", "w", "r")

    def __init__(self, name=""):
        self.name = name
        self.w = None
        self.r = {}


class Eng:
    def __init__(self, key, eng, sem):
        self.key = key
        self.eng = eng
        self.sem = sem
        self.cnt = 0
        self.pend = False
        self.seen = {}


class K:
    def __init__(self, nc, ndma_sems=8):
        self.nc = nc
        self.sems = {}
        self.stack = []

        def mk(name):
            cm = nc.semaphore("s_" + name)
            s = cm.__enter__()
            self.stack.append(cm)
            self.sems[name] = s
            return s
        self.pe = Eng("pe", nc.tensor, mk("pe"))
        self.act = Eng("act", nc.scalar, mk("act"))
        self.dve = Eng("dve", nc.vector, mk("dve"))
        self.pool = Eng("pool", nc.gpsimd, mk("pool"))
        self.sp = Eng("sp", nc.sync, mk("sp"))
        self.engs = [self.pe, self.act, self.dve, self.pool, self.sp]
        self.dpool = {}
        for q in ("sp", "pool", "act"):
            lst = []
            for i in range(ndma_sems):
                nm = "d_%s%d" % (q, i)
                mk(nm)
                lst.append([nm, 0])
            self.dpool[q] = [lst, 0]

    def close(self):
        for cm in reversed(self.stack):
            cm.__exit__(None, None, None)

    def _deps(self, E, reads, writes):
        deps = {}

        def add(ev, same_ok):
            if ev is None:
                return
            key, val = ev
            if key == E.key and not same_ok:
                return
            if deps.get(key, 0) < val:
                deps[key] = val
        for b in reads:
            add(b.w, True)
        for b in writes:
            add(b.w, False)
            for key, val in b.r.items():
                add((key, val), False)
        return deps

    def _wait(self, E, deps):
        for key, val in deps.items():
            if E.seen.get(key, 0) < val:
                if key == E.key and val > E.cnt:
                    raise RuntimeError("same-engine wait on un-inc'd event")
                E.eng.wait_ge(self.sems[key], val)
                E.seen[key] = val

    def op(self, E, fn, reads=(), writes=(), inc=True):
        self._wait(E, self._deps(E, reads, writes))
        inst = fn()
        if inc:
            E.cnt += 1
            inst.then_inc(E.sem, 1)
            E.pend = False
            val = E.cnt
        else:
            assert E is self.pe
            E.pend = True
            val = E.cnt + 1
        for b in reads:
            if b.r.get(E.key, 0) < val:
                b.r[E.key] = val
        for b in writes:
            b.w = (E.key, val)
            b.r = {}
        return inst

    def dma(self, Q, fn, reads=(), writes=()):
        self._wait(Q, self._deps(Q, reads, writes))
        lst, idx = self.dpool[Q.key]
        slot = lst[idx % len(lst)]
        self.dpool[Q.key][1] = idx + 1
        nm, cur = slot
        if cur > 0 and Q.seen.get(nm, 0) < cur:
            Q.eng.wait_ge(self.sems[nm], cur)
            Q.seen[nm] = cur
        inst = fn()
        cur += 16
        slot[1] = cur
        inst.then_inc(self.sems[nm], 16)
        for b in reads:
            if b.r.get(nm, 0) < cur:
                b.r[nm] = cur
        for b in writes:
            b.w = (nm, cur)
            b.r = {}
        return inst

    def barrier(self):
        targets = {}
        for E in self.engs:
            assert not E.pend
            if E.cnt:
                targets[E.key] = E.cnt
        for q, (lst, _) in self.dpool.items():
            for nm, cur in lst:
                if cur:
                    targets[nm] = cur
        for E in self.engs:
            for key, val in targets.items():
                if key == E.key:
                    continue
                if E.seen.get(key, 0) < val:
                    E.eng.wait_ge(self.sems[key], val)
                    E.seen[key] = val


class T:
    __slots__ = ("t", "b")

    def __init__(self, t, name):
        self.t = t
        self.b = Buf(name)


def build_nc(NSEQ, S, CAP, dbg=False):
    NT = S // 128
    TT = NSEQ * NT
    NSLOT = NEXP * CAP
    nc = bass.Bass("TRN2", target_bir_lowering=False)

    def din(name, shape, dt=F32):
        return nc.dram_tensor(name, list(shape), dt, kind="ExternalInput").ap()

    x_d = din("x", [NSEQ * S, D])
    mem_d = din("mem", [NSEQ * 256, D])
    w_in_d = din("w_in", [D, 3072])
    w_out_d = din("w_out", [D, D])
    w_xq_d = din("w_xq", [D, D])
    w_xkv_d = din("w_xkv", [D, 2 * D])
    w_xo_d = din("w_xo", [D, D])
    w_eg_d = din("w_eg", [NEXP, D, 512])
    w_eu_d = din("w_eu", [NEXP, D, 512])
    w_ed_d = din("w_ed", [NEXP, 512, D])
    w_r_d = din("w_r", [D, 68])
    b_r_d = din("b_r", [1, 68])
    gcols_d = din("gcols", [128, 32])
    lng_d = din("lng", [1, 512])
    lbl_d = din("lbl", [2, 512])
    gffn_d = din("gffn", [1, D])
    gfin_d = din("gfin", [1, D])
    wspT_d = din("wspT", [128, 4, 128])
    bspT_d = din("bspT", [128, 4])
    cst_d = din("cst", [128, 674])
    out_d = nc.dram_tensor("out", [NSEQ * S, D], F32, kind="ExternalOutput").ap()
    if dbg:
        dbg_h1 = nc.dram_tensor("dbg_h1", [NSEQ * S, D], F32, kind="ExternalOutput").ap()
        dbg_h2 = nc.dram_tensor("dbg_h2", [NSEQ * S, D], F32, kind="ExternalOutput").ap()
        dbg_yc = nc.dram_tensor("dbg_yc", [NSEQ * S, D], F32, kind="ExternalOutput").ap()
    xs_d = nc.dram_tensor("xs_scr", [NSLOT, D], BF16, kind="Internal").ap()
    yb_d = nc.dram_tensor("yb_scr", [NSLOT, D], BF16, kind="Internal").ap()
    hs_d = nc.dram_tensor("hs_scr", [TT * 128, D], F32, kind="Internal").ap()
    kv_d = nc.dram_tensor("kv_scr", [NSEQ, 128, 4096], BF16, kind="Internal").ap()

    k = K(nc)
    pe, act, dve, pool, sp = k.pe, k.act, k.dve, k.pool, k.sp
    V_, S_, G_, TE = nc.vector, nc.scalar, nc.gpsimd, nc.tensor

    scopes = [[]]

    def sb(name, shape, dt):
        cm = nc.sbuf_tensor("sb_" + name, list(shape), dt)
        t = cm.__enter__()
        scopes[-1].append(cm)
        return T(t, name)

    def sbn(name, shape, dt, n):
        return [sb("%s%d" % (name, i), shape, dt) for i in range(n)]

    def push_scope():
        scopes.append([])

    def pop_scope():
        for cm in reversed(scopes.pop()):
            cm.__exit__(None, None, None)

    banks = []
    for i in range(8):
        cm = nc.psum_tensor("ps%d" % i, [128, 512], F32)
        banks.append(T(cm.__enter__(), "ps%d" % i))
        scopes[0].append(cm)
    bank_i = [0]

    def ps():
        b = banks[bank_i[0] % 8]
        bank_i[0] += 1
        return b

    cstp = sb("cstp", [128, 162], F32)
    ident = sb("ident", [128, 128], BF16)
    ones_bf = sb("ones_bf", [128, 128], BF16)
    slt_bf = sb("slt_bf", [128, 128], BF16)
    dest = sb("dest", [128, TT, 2], I32)
    gates = sb("gates", [128, TT, 2], F32)
    nhalf = sb("nhalf", [128, 8], F32)
    C_ID, C_LE, C_LT, C_ONE, C_MX, C_EC = 0, 128, 256, 384, 512, 642
    mext = cstp.t[:, 0:130]
    ecap = cstp.t[:, 130:162]
    cst = cstp

    k.dma(sp, lambda: nc.sync.dma_start(out=cstp.t[:], in_=cst_d[:, C_MX:C_MX + 162]), writes=[cstp.b])
    k.op(pool, lambda: G_.memset(nhalf.t[:], -0.5), [], [nhalf.b])
    k.op(pool, lambda: G_.memset(nhalf.t[:, 7:8], -1.0), [], [nhalf.b])

    def rsqrt_col(dst, dst_buf, src, src_buf, scale, ncol=1):
        k.op(pool, lambda: G_.tensor_scalar(out=dst, in0=src, scalar1=scale, scalar2=EPS, op0=ALU.mult, op1=ALU.add),
             [src_buf], [dst_buf])
        k.op(pool, lambda: G_.tensor_tensor(out=dst, in0=dst, in1=nhalf.t[:, 0:ncol], op=ALU.pow),
             [dst_buf, nhalf.b], [dst_buf])

    push_scope()
    w_in = sb("w_in", [128, 8, 3072], BF16)
    w_out = sb("w_out", [128, 8, 1024], BF16)
    w_xq = sb("w_xq", [128, 8, 1024], BF16)
    w_xo = sb("w_xo", [128, 8, 1024], BF16)
    w_r = sb("w_r", [128, 8, 68], BF16)
    gcols = sb("gcols", [128, 32], F32)
    lng_b = sb("lng_b", [128, 512], F32)
    lb_b = sb("lb_b", [128, 512], F32)
    omlb_b = sb("omlb_b", [128, 512], F32)
    gffn_b = sb("gffn_b", [128, D], F32)
    br_b = sb("br_b", [128, 68], F32)
    wsT = sb("wsT", [128, 4, 128], BF16)
    bspT = sb("bspT", [128, 4], F32)
    negm4 = sb("negm4", [128, 4, 128], BF16)
    Macc = sb("Macc", [128, 32], BF16)
    Sst = sb("Sst", [128, 512], F32)

    k.dma(sp, lambda: nc.sync.dma_start(out=gcols.t[:], in_=gcols_d[:, :]), writes=[gcols.b])
    k.dma(sp, lambda: nc.sync.dma_start(out=lng_b.t[:], in_=lng_d[0:1, :].partition_broadcast(128)), writes=[lng_b.b])
    k.dma(sp, lambda: nc.sync.dma_start(out=lb_b.t[:], in_=lbl_d[0:1, :].partition_broadcast(128)), writes=[lb_b.b])
    k.dma(sp, lambda: nc.sync.dma_start(out=omlb_b.t[:], in_=lbl_d[1:2, :].partition_broadcast(128)), writes=[omlb_b.b])
    k.dma(sp, lambda: nc.sync.dma_start(out=gffn_b.t[:], in_=gffn_d[0:1, :].partition_broadcast(128)), writes=[gffn_b.b])
    k.dma(sp, lambda: nc.sync.dma_start(out=br_b.t[:], in_=b_r_d[0:1, :].partition_broadcast(128)), writes=[br_b.b])
    k.dma(sp, lambda: nc.sync.dma_start(out=bspT.t[:], in_=bspT_d[:, :]), writes=[bspT.b])
    k.dma(pool, lambda: G_.dma_start(out=w_r.t[:], in_=w_r_d.rearrange("(kc p) n -> p kc n", p=128)), writes=[w_r.b])
    k.dma(pool, lambda: G_.dma_start(out=w_xo.t[:], in_=w_xo_d.rearrange("(kc p) n -> p kc n", p=128)), writes=[w_xo.b])
    k.op(dve, lambda: V_.tensor_tensor(out=lb_b.t[:], in0=lb_b.t[:], in1=omlb_b.t[:], op=ALU.subtract), [lb_b.b, omlb_b.b], [lb_b.b])
    k.op(act, lambda: S_.activation(out=lb_b.t[:], in_=lb_b.t[:], func=AF.Sigmoid), [lb_b.b], [lb_b.b])
    k.op(dve, lambda: V_.tensor_scalar(out=omlb_b.t[:], in0=lb_b.t[:], scalar1=-1.0, scalar2=1.0, op0=ALU.mult, op1=ALU.add),
         [lb_b.b], [omlb_b.b])
    k.op(pool, lambda: G_.memset(Macc.t[:], 0.0), [], [Macc.b])
    k.op(pool, lambda: G_.memset(Sst.t[:], 0.0), [], [Sst.b])

    push_scope()
    cstb = sb("cstb", [128, 512], F32)
    k.dma(sp, lambda: nc.sync.dma_start(out=cstb.t[:], in_=cst_d[:, 0:512]), writes=[cstb.b])
    k.op(dve, lambda: V_.tensor_copy(out=ident.t[:], in_=cstb.t[:, C_ID:C_ID + 128]), [cstb.b], [ident.b])
    k.op(dve, lambda: V_.tensor_copy(out=ones_bf.t[:], in_=cstb.t[:, C_ONE:C_ONE + 128]), [cstb.b], [ones_bf.b])
    k.op(dve, lambda: V_.tensor_copy(out=slt_bf.t[:], in_=cstb.t[:, C_LT:C_LT + 128]), [cstb.b], [slt_bf.b])
    for h in range(4):
        k.op(dve, lambda h=h: V_.tensor_scalar(out=negm4.t[:, h, :], in0=cstb.t[:, C_LE:C_LE + 128], scalar1=-1.0, scalar2=None, op0=ALU.mult),
             [cstb.b], [negm4.b])
    stg = sbn("stg", [128, 2048], F32, 2)
    stg_i = [0]

    def load_scaled(dst, dst_w, src_d, ncols, gc0):
        npc = 1 if ncols <= 2048 else 2
        pw = ncols // npc
        for kc in range(8):
            for pc in range(npc):
                st = stg[stg_i[0] % 2]
                stg_i[0] += 1
                c0 = pc * pw
                k.dma(sp, lambda: nc.sync.dma_start(out=st.t[:, 0:pw], in_=src_d[kc * 128:(kc + 1) * 128, c0:c0 + pw]), writes=[st.b])
                if stg_i[0] % 2 == 0:
                    k.op(dve, lambda: V_.tensor_scalar(out=dst.t[:, kc, c0:c0 + pw], in0=st.t[:, 0:pw], scalar1=gcols.t[:, gc0 + kc:gc0 + kc + 1], scalar2=None, op0=ALU.mult),
                         [st.b, gcols.b], [dst.b])
                else:
                    k.op(act, lambda: S_.activation(out=dst.t[:, kc, c0:c0 + pw], in_=st.t[:, 0:pw], func=AF.Copy, scale=gcols.t[:, gc0 + kc:gc0 + kc + 1]),
                         [st.b, gcols.b], [dst.b])
                yield

    w_xkv = sb("w_xkv", [128, 8, 2048], BF16)
    for _ in load_scaled(w_xkv, None, w_xkv_d, 2048, 16):
        pass

    def gen_weights():
        yield from load_scaled(w_in, None, w_in_d, 3072, 0)
        yield from load_scaled(w_out, None, w_out_d, 1024, 24)
        yield from load_scaled(w_xq, None, w_xq_d, 1024, 8)

    wsf = sb("wsf", [128, 4, 128], F32)
    k.dma(sp, lambda: nc.sync.dma_start(out=wsf.t[:], in_=wspT_d[:, :, :]), writes=[wsf.b])
    for g in range(4):
        k.op(dve, lambda g=g: V_.tensor_tensor(out=wsT.t[:, g, :], in0=wsf.t[:, g, :], in1=cstb.t[:, C_LE:C_LE + 128], op=ALU.mult),
             [wsf.b, cstb.b], [wsT.b])

    def sumsq(src_ap, src_buf, junk, col_ap, col_buf, n=D):
        k.op(act, lambda: S_.activation(out=junk.t[:, 0:n], in_=src_ap, func=AF.Square, accum_out=col_ap),
             [src_buf], [junk.b, col_buf])

    mt = sbn("memt", [128, D], F32, 2)
    mjunk = sb("mjunk", [128, D], BF16)
    mn = sbn("memn", [128, D], BF16, 2)
    mnT = sb("memnT", [128, 8, 256], BF16)
    mcol = sbn("mcol", [128, 1], F32, 2)
    kvs = sbn("kvs", [128, 4096], BF16, 1)
    kv_db = [Buf("kvd%d" % s) for s in range(NSEQ)]
    def gen_kv():
      for s in range(NSEQ):
          kvt = kvs[0]
          for mc in range(2):
              m_ = mt[mc]
              k.dma(pool, lambda m_=m_, mc=mc, s=s: G_.dma_start(out=m_.t[:], in_=mem_d[s * 256 + mc * 128: s * 256 + (mc + 1) * 128, :]), writes=[m_.b])
              sumsq(m_.t[:], m_.b, mjunk, mcol[mc].t[:, 0:1], mcol[mc].b)
              rsqrt_col(mcol[mc].t[:, 0:1], mcol[mc].b, mcol[mc].t[:, 0:1], mcol[mc].b, 1.0 / D)
              k.op(dve, lambda m_=m_, mc=mc: V_.tensor_scalar(out=mn[mc].t[:], in0=m_.t[:], scalar1=mcol[mc].t[:, 0:1], scalar2=None, op0=ALU.mult),
                   [m_.b, mcol[mc].b], [mn[mc].b])
              bk = ps()
              pv = bk.t[:].bitcast(BF16)
              for kc in range(8):
                  k.op(pe, lambda kc=kc, pv=pv, mc=mc: TE.transpose(out=pv[:, kc * 128:(kc + 1) * 128], in_=mn[mc].t[:, kc * 128:(kc + 1) * 128], identity=ident.t[:]),
                       [mn[mc].b, ident.b], [bk.b], inc=(kc == 7))
              k.op(act, lambda pv=pv, mc=mc: S_.copy(out=mnT.t[:, :, mc * 128:(mc + 1) * 128], in_=pv.rearrange("p (a b) -> p a b", a=8)),
                   [bk.b], [mnT.b])
              yield
          for c in range(8):
              bk = ps()
              for kc in range(8):
                  k.op(pe, lambda kc=kc, c=c, bk=bk: TE.matmul(bk.t[:, 0:256], lhsT=w_xkv.t[:, kc, c * 128:(c + 1) * 128], rhs=mnT.t[:, kc, :], start=(kc == 0), stop=(kc == 7)),
                       [w_xkv.b, mnT.b], [bk.b], inc=(kc == 7))
              if c % 2 == 0:
                  k.op(dve, lambda c=c, bk=bk, kvt=kvt: V_.tensor_copy(out=kvt.t[:, c * 256:(c + 1) * 256], in_=bk.t[:, 0:256]), [bk.b], [kvt.b])
              else:
                  k.op(act, lambda c=c, bk=bk, kvt=kvt: S_.copy(out=kvt.t[:, c * 256:(c + 1) * 256], in_=bk.t[:, 0:256]), [bk.b], [kvt.b])
              yield
          for mc in range(2):
              for hf in range(2):
                  bk = ps()
                  for kc in range(8):
                      k.op(pe, lambda kc=kc, mc=mc, hf=hf, bk=bk: TE.matmul(bk.t[:, :], lhsT=mnT.t[:, kc, mc * 128:(mc + 1) * 128], rhs=w_xkv.t[:, kc, 1024 + hf * 512:1024 + (hf + 1) * 512], start=(kc == 0), stop=(kc == 7)),
                           [w_xkv.b, mnT.b], [bk.b], inc=(kc == 7))
                  o0 = 2048 + mc * 1024 + hf * 512
                  if hf == 0:
                      k.op(dve, lambda bk=bk, kvt=kvt, o0=o0: V_.tensor_copy(out=kvt.t[:, o0:o0 + 512], in_=bk.t[:, :]), [bk.b], [kvt.b])
                  else:
                      k.op(act, lambda bk=bk, kvt=kvt, o0=o0: S_.copy(out=kvt.t[:, o0:o0 + 512], in_=bk.t[:, :]), [bk.b], [kvt.b])
                  yield
          k.dma(sp, lambda s=s, kvt=kvt: nc.sync.dma_start(out=kv_d[s, :, :], in_=kvt.t[:]), reads=[kvt.b], writes=[kv_db[s]])
    gens_ = [gen_weights(), gen_kv()]
    while gens_:
        for g_ in list(gens_):
            try:
                next(g_)
            except StopIteration:
                gens_.remove(g_)
    k.barrier()
    pop_scope()

    NX = 5
    Xs = sbn("X", [128, D], F32, NX)
    xnA = sbn("xnA", [128, D], BF16, 1)
    xnTA = sbn("xnTA", [128, 8, 128], BF16, 1)
    xnC = sbn("xnC", [128, D], BF16, 1)
    xnTC = sbn("xnTC", [128, 8, 128], BF16, 1)
    xnD = sbn("xnD", [128, D], BF16, 1)
    xnTD = sbn("xnTD", [128, 8, 128], BF16, 1)
    ncol = sbn("ncol", [128, 16], F32, 8)
    ug = sbn("ug", [128, 512], F32, 2)
    vg = sbn("vg", [128, 512], F32, 1)
    vn = sbn("vn", [128, 512], BF16, 2)
    vst = sbn("vst", [128, 8], F32, 2)
    fS = sbn("fS", [128, 512], F32, 1)
    logf = sbn("logf", [128, 512], F32, 1)
    kinv = sbn("kinv", [128, 512], BF16, 2)
    kinvT = sbn("kinvT", [128, 512], BF16, 2)
    sq = sbn("sq", [128, 512], F32, 1)
    ebT = sbn("ebT", [128, 512], F32, 1)
    cde = sbn("cde", [128, 12], F32, 2)
    qdT = sbn("qdT", [128, 512], BF16, 2)
    vb = sbn("vb", [128, 512], BF16, 2)
    gs = sbn("gs", [128, 512], F32, 2)
    scT = sbn("scT", [128, 512], BF16, 2)
    Sp = sbn("Sp", [128, 512], BF16, 2)
    yy = sbn("yy", [128, 512], F32, 1)
    ycat = sbn("ycat", [128, D], BF16, 2)
    ycatT = sbn("ycatT", [128, 8, 128], BF16, 1)
    qTa = sbn("qTa", [128, 8, 128], BF16, 2)
    ET = sbn("ET", [128, 2, 4, 128], BF16, 1)
    Rinv = sbn("Rinv", [128, 4, 128], F32, 1)
    oT = sbn("oT", [128, 8, 128], BF16, 1)
    lg = sbn("lg", [128, 68], F32, 2)
    rt = sbn("rt", [128, 160], F32, 2)
    Mb = sbn("Mb", [128, 32], BF16, 2)
    kvr = sbn("kvr", [128, 4096], BF16, 1)

    dest_b = [Buf("dest%d" % i) for i in range(TT)]
    gates_b = [Buf("gates%d" % i) for i in range(TT)]
    x_db = Buf("x_dram")
    hs_db = [Buf("hs%d" % i) for i in range(TT)]
    xs_db = [Buf("xs%d" % i) for i in range(2 * TT)]
    dbg_db = Buf("dbg")

    def mm_group(bk, out_ap, pairs, reads):
        n = len(pairs)
        for j, (l, r) in enumerate(pairs):
            k.op(pe, lambda l=l, r=r, j=j: TE.matmul(out_ap, lhsT=l, rhs=r, start=(j == 0), stop=(j == n - 1)),
                 reads, [bk.b], inc=(j == n - 1))

    def dbg_dump(src, src_buf, dst_d, i, bf=False):
        if not dbg:
            return
        if bf:
            return
            d_ = dbt[i % 2]
            k.op(dve, lambda: V_.tensor_copy(out=d_.t[:], in_=src), [src_buf], [d_.b])
            k.dma(sp, lambda: nc.sync.dma_start(out=dst_d[i * 128:(i + 1) * 128, :], in_=d_.t[:]), reads=[d_.b], writes=[dbg_db])
        else:
            k.dma(sp, lambda: nc.sync.dma_start(out=dst_d[i * 128:(i + 1) * 128, :], in_=src), reads=[src_buf], writes=[dbg_db])

    stage_banks = {"A": [0, 1, 2], "B": [3, 4], "C1": [5], "C2": [6], "D": [7]}
    stage_bi = {kk: 0 for kk in stage_banks}

    def psS(st):
        lst = stage_banks[st]
        b = banks[lst[stage_bi[st] % len(lst)]]
        stage_bi[st] += 1
        return b

    def norm_T(st, X, xn_t, xnT_t, col, gain=None):
        sumsq(X.t[:], X.b, xn_t, col.t[:, 0:1], col.b)
        rsqrt_col(col.t[:, 0:1], col.b, col.t[:, 0:1], col.b, 1.0 / D)
        yield
        if gain is None:
            k.op(dve, lambda: V_.tensor_scalar(out=xn_t.t[:], in0=X.t[:], scalar1=col.t[:, 0:1], scalar2=None, op0=ALU.mult),
                 [X.b, col.b], [xn_t.b])
        else:
            k.op(dve, lambda: V_.scalar_tensor_tensor(out=xn_t.t[:], in0=X.t[:], scalar=col.t[:, 0:1], in1=gain.t[:], op0=ALU.mult, op1=ALU.mult),
                 [X.b, col.b, gain.b], [xn_t.b])
        yield
        bk = psS(st)
        pv = bk.t[:].bitcast(BF16)
        for kc in range(8):
            k.op(pe, lambda kc=kc: TE.transpose(out=pv[:, kc * 128:(kc + 1) * 128], in_=xn_t.t[:, kc * 128:(kc + 1) * 128], identity=ident.t[:]),
                 [xn_t.b, ident.b], [bk.b], inc=(kc == 7))
        yield
        k.op(act, lambda: S_.copy(out=xnT_t.t[:].rearrange("p a b -> p (a b)"), in_=pv), [bk.b], [xnT_t.b])
        yield

    def stage_A(i):
        X = Xs[i % NX]
        s2 = i % 2
        k.dma(sp, lambda: nc.sync.dma_start(out=X.t[:], in_=x_d[i * 128:(i + 1) * 128, :]), reads=[x_db], writes=[X.b])
        col = ncol[i % 8]
        xT = xnTA[0]
        yield from norm_T("A", X, xnA[0], xT, col)
        ug_, vg_, vn_, fS_, lf_, vb_, gs_, sq_ = ug[s2], vg[0], vn[s2], fS[0], logf[0], vb[s2], gs[s2], sq[0]

        def tok_group(c):
            bk = psS("A")
            mm_group(bk, bk.t[:, :], [(xT.t[:, kc, :], w_in.t[:, kc, c * 512:(c + 1) * 512]) for kc in range(8)], [xT.b, w_in.b])
            return bk
        b_f = tok_group(3)
        yield
        b_i = tok_group(4)
        k.op(act, lambda: S_.activation(out=fS_.t[:], in_=b_f.t[:, :], func=AF.Sigmoid), [b_f.b], [fS_.b])
        yield
        b_v = tok_group(1)
        k.op(dve, lambda: V_.tensor_copy(out=vb_.t[:], in_=b_i.t[:, :]), [b_i.b], [vb_.b])
        k.op(pool, lambda: G_.tensor_tensor(out=fS_.t[:], in0=fS_.t[:], in1=omlb_b.t[:], op=ALU.mult), [fS_.b, omlb_b.b], [fS_.b])
        k.op(pool, lambda: G_.tensor_tensor(out=fS_.t[:], in0=fS_.t[:], in1=lb_b.t[:], op=ALU.add), [fS_.b, lb_b.b], [fS_.b])
        yield
        b_u = tok_group(0)
        k.op(act, lambda: S_.activation(out=vg_.t[:], in_=b_v.t[:, :], func=AF.Gelu_apprx_tanh), [b_v.b], [vg_.b])
        yield
        b_g = tok_group(5)
        k.op(act, lambda: S_.activation(out=ug_.t[:], in_=b_u.t[:, :], func=AF.Gelu_apprx_tanh), [b_u.b], [ug_.b])
        k.op(act, lambda: S_.activation(out=lf_.t[:], in_=fS_.t[:], func=AF.Ln), [fS_.b], [lf_.b])
        st_ = vst[s2]
        k.op(dve, lambda: V_.bn_stats(out=st_.t[:, 0:6], in_=vg_.t[:]), [vg_.b], [st_.b])
        k.op(dve, lambda: V_.bn_aggr(out=col.t[:, 2:4], in_=st_.t[:, 0:6]), [st_.b], [col.b])
        rsqrt_col(col.t[:, 3:4], col.b, col.t[:, 3:4], col.b, 1.0)
        yield
        b_q = psS("A")
        for h in range(4):
            mm_group(b_q, b_q.t[:, h * 128:(h + 1) * 128],
                     [(w_in.t[:, kc, 1024 + h * 128:1024 + (h + 1) * 128], xT.t[:, kc, :]) for kc in range(8)], [xT.b, w_in.b])
        k.op(act, lambda: S_.activation(out=gs_.t[:], in_=b_g.t[:, :], func=AF.Silu), [b_g.b], [gs_.b])
        k.op(dve, lambda: V_.scalar_tensor_tensor(out=col.t[:, 4:5], in0=col.t[:, 2:3], scalar=-1.0, in1=col.t[:, 3:4], op0=ALU.mult, op1=ALU.mult),
             [col.b], [col.b])
        yield
        b_b = psS("A")
        k.op(pe, lambda: TE.matmul(b_b.t[:, :], lhsT=mext[:, 0:128], rhs=lf_.t[:, :], start=True, stop=True), [cst.b, lf_.b], [b_b.b])
        k.op(act, lambda: S_.activation(out=sq_.t[:], in_=b_q.t[:, :], func=AF.Silu), [b_q.b], [sq_.b])
        k.op(act, lambda: S_.activation(out=vg_.t[:], in_=vg_.t[:], func=AF.Identity, scale=col.t[:, 3:4], bias=col.t[:, 4:5]),
             [vg_.b, col.b], [vg_.b])
        yield
        b_bT = psS("A")
        for h in range(4):
            k.op(pe, lambda h=h: TE.matmul(b_bT.t[:, h * 128:(h + 1) * 128], lhsT=lf_.t[:, h * 128:(h + 1) * 128], rhs=mext[:, 0:128], start=True, stop=True),
                 [cst.b, lf_.b], [b_bT.b], inc=(h == 3))
        k.op(dve, lambda: V_.tensor_tensor(out=vn_.t[:], in0=vg_.t[:], in1=lng_b.t[:], op=ALU.mult), [vg_.b, lng_b.b], [vn_.b])
        yield
        b_cd = psS("A")
        for h in range(4):
            k.op(pe, lambda h=h: TE.matmul(b_cd.t[:, h * 2:(h + 1) * 2], lhsT=lf_.t[:, h * 128:(h + 1) * 128], rhs=mext[:, 128:130], start=True, stop=True),
                 [cst.b, lf_.b], [b_cd.b], inc=(h == 3))
        en_, ki_, kT_, eb_, cd_, qd_ = logf[0], kinv[s2], kinvT[s2], ebT[0], cde[s2], qdT[s2]
        k.op(act, lambda: S_.activation(out=eb_.t[:], in_=b_bT.t[:, :], func=AF.Exp), [b_bT.b], [eb_.b])
        yield
        k.op(act, lambda: S_.activation(out=cd_.t[:, 0:8], in_=b_cd.t[:, 0:8], func=AF.Exp), [b_cd.b], [cd_.b])
        k.op(act, lambda: S_.activation(out=en_.t[:], in_=b_b.t[:, :], func=AF.Exp, scale=-1.0), [b_b.b], [en_.b])
        k.op(dve, lambda: V_.tensor_copy(out=cd_.t[:, 8:12], in_=eb_.t[:].rearrange("p (h t) -> p h t", h=4)[:, :, 127]), [eb_.b], [cd_.b])
        k.op(dve, lambda: V_.tensor_tensor(out=qd_.t[:], in0=sq_.t[:], in1=eb_.t[:], op=ALU.mult), [sq_.b, eb_.b], [qd_.b])
        yield
        k.op(dve, lambda: V_.scalar_tensor_tensor(out=ki_.t[:], in0=fS_.t[:], scalar=1.0, in1=en_.t[:], op0=ALU.subtract, op1=ALU.mult),
             [fS_.b, en_.b], [ki_.b])
        yield
        bk = psS("A")
        pv = bk.t[:].bitcast(BF16)
        for h in range(4):
            k.op(pe, lambda h=h: TE.transpose(out=pv[:, h * 128:(h + 1) * 128], in_=ki_.t[:, h * 128:(h + 1) * 128], identity=ident.t[:]),
                 [ki_.b, ident.b], [bk.b], inc=(h == 3))
        yield
        k.op(act, lambda: S_.copy(out=kT_.t[:], in_=pv[:, 0:512]), [bk.b], [kT_.b])

    def stage_B(i):
        s2 = i % 2
        col = ncol[i % 8]
        ug_, vn_, vb_, gs_ = ug[s2], vn[s2], vb[s2], gs[s2]
        ki_, kT_, cd_, qd_ = kinv[s2], kinvT[s2], cde[s2], qdT[s2]
        sc_, Sp_, yc_, yy_, ut_ = scT[s2], Sp[s2], ycat[s2], yy[0], yy[0]
        if i % NT == 0 and i > 0:
            k.op(pool, lambda: G_.memset(Sst.t[:], 0.0), [], [Sst.b])
        b_s = psS("B")
        for h in range(4):
            k.op(pe, lambda h=h: TE.matmul(b_s.t[:, h * 128:(h + 1) * 128], lhsT=kT_.t[:, h * 128:(h + 1) * 128], rhs=qd_.t[:, h * 128:(h + 1) * 128], start=True, stop=True),
                 [kT_.b, qd_.b], [b_s.b], inc=(h == 3))
        b_z = psS("B")
        for g in range(4):
            k.op(pe, lambda g=g: TE.matmul(b_z.t[:, g * 128:(g + 1) * 128], lhsT=wsT.t[:, g, :], rhs=vn_.t[:, g * 128:(g + 1) * 128], start=True, stop=True),
                 [wsT.b, vn_.b], [b_z.b], inc=(g == 3))
        v4 = lambda ap: ap.rearrange("p (h v) -> p h v", h=4)
        cdv = cd_.t[:, 0:8].rearrange("p (h two) -> p h two", two=2)
        k.op(dve, lambda: V_.tensor_tensor(out=v4(Sp_.t[:]), in0=v4(Sst.t[:]), in1=cdv[:, :, 0:1].to_broadcast([128, 4, 128]), op=ALU.mult),
             [Sst.b, cd_.b], [Sp_.b])
        yield
        k.op(dve, lambda: V_.tensor_tensor(out=sc_.t[:], in0=b_s.t[:, :], in1=negm4.t[:].rearrange("p a b -> p (a b)"), op=ALU.mult),
             [b_s.b, negm4.b], [sc_.b])
        for g in range(4):
            k.op(dve, lambda g=g: V_.scalar_tensor_tensor(out=yy_.t[:, g * 128:(g + 1) * 128], in0=b_z.t[:, g * 128:(g + 1) * 128], scalar=bspT.t[:, g:g + 1],
                                                          in1=ug_.t[:, g * 128:(g + 1) * 128], op0=ALU.add, op1=ALU.mult),
                 [b_z.b, bspT.b, ug_.b], [yy_.b])
        yield
        b_o = psS("B")
        for h in range(4):
            hs_ = slice(h * 128, (h + 1) * 128)
            k.op(pe, lambda hs_=hs_: TE.matmul(b_o.t[:, hs_], lhsT=sc_.t[:, hs_], rhs=vb_.t[:, hs_], start=True, stop=False),
                 [sc_.b, vb_.b], [b_o.b], inc=False)
            k.op(pe, lambda hs_=hs_: TE.matmul(b_o.t[:, hs_], lhsT=qd_.t[:, hs_], rhs=Sp_.t[:, hs_], start=False, stop=True),
                 [qd_.b, Sp_.b], [b_o.b], inc=(h == 3))
        b_U = psS("B")
        for h in range(4):
            hs_ = slice(h * 128, (h + 1) * 128)
            k.op(pe, lambda hs_=hs_: TE.matmul(b_U.t[:, hs_], lhsT=ki_.t[:, hs_], rhs=vb_.t[:, hs_], start=True, stop=True),
                 [ki_.b, vb_.b], [b_U.b], inc=(h == 3))
        sumsq(yy_.t[:], yy_.b, yc_, col.t[:, 5:6], col.b, n=512)
        rsqrt_col(col.t[:, 5:6], col.b, col.t[:, 5:6], col.b, 1.0 / 512)
        yield
        k.op(dve, lambda: V_.tensor_scalar(out=yc_.t[:, 0:512], in0=yy_.t[:], scalar1=col.t[:, 5:6], scalar2=None, op0=ALU.mult),
             [yy_.b, col.b], [yc_.b])
        for h in range(4):
            hs_ = slice(h * 128, (h + 1) * 128)
            k.op(act, lambda hs_=hs_, h=h: S_.activation(out=yc_.t[:, 512 + h * 128:512 + (h + 1) * 128], in_=b_o.t[:, hs_], func=AF.Square, accum_out=col.t[:, 8 + h:9 + h]),
                 [b_o.b], [yc_.b, col.b])
        rsqrt_col(col.t[:, 8:12], col.b, col.t[:, 8:12], col.b, 1.0 / 128, ncol=4)
        yield
        k.op(dve, lambda: V_.tensor_tensor(out=v4(yy_.t[:]), in0=v4(b_o.t[:, :]), in1=col.t[:, 8:12].unsqueeze(2).to_broadcast([128, 4, 128]), op=ALU.mult),
             [b_o.b, col.b], [yy_.b])
        k.op(dve, lambda: V_.tensor_tensor(out=yc_.t[:, 512:1024], in0=yy_.t[:], in1=gs_.t[:], op=ALU.mult), [yy_.b, gs_.b], [yc_.b])
        k.op(dve, lambda: V_.tensor_tensor(out=v4(ut_.t[:]), in0=v4(b_U.t[:, :]), in1=cd_.t[:, 8:12].unsqueeze(2).to_broadcast([128, 4, 128]), op=ALU.mult),
             [b_U.b, cd_.b], [ut_.b])
        k.op(dve, lambda: V_.tensor_tensor(out=v4(Sst.t[:]), in0=v4(Sst.t[:]), in1=cdv[:, :, 1:2].to_broadcast([128, 4, 128]), op=ALU.mult),
             [Sst.b, cd_.b], [Sst.b])
        k.op(dve, lambda: V_.tensor_tensor(out=Sst.t[:], in0=Sst.t[:], in1=ut_.t[:], op=ALU.subtract), [Sst.b, ut_.b], [Sst.b])

    def stage_C(i):
        X = Xs[i % NX]
        s2 = i % 2
        col = ncol[i % 8]
        yc_, ycT_ = ycat[s2], ycatT[0]
        bk = psS("C1")
        pv = bk.t[:].bitcast(BF16)
        for kc in range(8):
            k.op(pe, lambda kc=kc: TE.transpose(out=pv[:, kc * 128:(kc + 1) * 128], in_=yc_.t[:, kc * 128:(kc + 1) * 128], identity=ident.t[:]),
                 [yc_.b, ident.b], [bk.b], inc=(kc == 7))
        yield
        k.op(act, lambda: S_.copy(out=ycT_.t[:].rearrange("p a b -> p (a b)"), in_=pv), [bk.b], [ycT_.b])
        yield
        for hf in range(2):
            bk = psS("C1")
            mm_group(bk, bk.t[:, :], [(ycT_.t[:, kc, :], w_out.t[:, kc, hf * 512:(hf + 1) * 512]) for kc in range(8)], [ycT_.b, w_out.b])
            yield
            k.op(dve, lambda hf=hf, bk=bk: V_.tensor_tensor(out=X.t[:, hf * 512:(hf + 1) * 512], in0=X.t[:, hf * 512:(hf + 1) * 512], in1=bk.t[:, :], op=ALU.add),
                 [X.b, bk.b], [X.b])
        dbg_dump(X.t[:], X.b, dbg_h1 if dbg else None, i)
        xT = xnTC[0]
        yield from norm_T("C1", X, xnC[0], xT, col)
        qT_ = qTa[s2]
        for half in range(2):
            bk = psS("C1")
            for cc in range(4):
                c = half * 4 + cc
                mm_group(bk, bk.t[:, cc * 128:(cc + 1) * 128], [(w_xq.t[:, kc, c * 128:(c + 1) * 128], xT.t[:, kc, :]) for kc in range(8)], [xT.b, w_xq.b])
            yield
            if half == 0:
                k.op(act, lambda bk=bk: S_.copy(out=qT_.t[:, 0:4, :].rearrange("p a b -> p (a b)"), in_=bk.t[:, :]), [bk.b], [qT_.b])
            else:
                k.op(dve, lambda bk=bk: V_.tensor_copy(out=qT_.t[:, 4:8, :].rearrange("p a b -> p (a b)"), in_=bk.t[:, :]), [bk.b], [qT_.b])

    def stage_C2(i):
        X = Xs[i % NX]
        s2 = i % 2
        kv_ = kvr[0]
        qT_ = qTa[s2]
        if i % NT == 0:
            s = i // NT
            k.dma(sp, lambda: nc.sync.dma_start(out=kv_.t[:], in_=kv_d[s, :, :]), reads=[kv_db[s]], writes=[kv_.b])
        ET_ = ET[0]
        for mc in range(2):
            bk = psS("C2")
            for h in range(4):
                mm_group(bk, bk.t[:, h * 128:(h + 1) * 128],
                         [(kv_.t[:, (2 * h + dc) * 256 + mc * 128:(2 * h + dc) * 256 + (mc + 1) * 128], qT_.t[:, 2 * h + dc, :]) for dc in range(2)], [kv_.b, qT_.b])
            yield
            k.op(act, lambda bk=bk, mc=mc: S_.activation(out=ET_.t[:, mc, :, :].rearrange("p a b -> p (a b)"), in_=bk.t[:, :], func=AF.Exp, scale=1.0 / 16.0),
                 [bk.b], [ET_.b])
        yield
        b_r = psS("C2")
        mm_group(b_r, b_r.t[:, :], [(ones_bf.t[:], ET_.t[:, mc, :, :].rearrange("p a b -> p (a b)")) for mc in range(2)], [ones_bf.b, ET_.b])
        yield
        Ri = Rinv[0]
        k.op(dve, lambda: V_.reciprocal(out=Ri.t[:].rearrange("p a b -> p (a b)"), in_=b_r.t[:, :]), [b_r.b], [Ri.b])
        oT_ = oT[0]
        for half in range(2):
            bk = psS("C2")
            for cc in range(4):
                c = half * 4 + cc
                h, dc = c // 2, c % 2
                mm_group(bk, bk.t[:, cc * 128:(cc + 1) * 128],
                         [(kv_.t[:, 2048 + mc * 1024 + h * 256 + dc * 128:2048 + mc * 1024 + h * 256 + (dc + 1) * 128], ET_.t[:, mc, h, :]) for mc in range(2)], [kv_.b, ET_.b])
            yield
            k.op(dve, lambda bk=bk, half=half: V_.tensor_tensor(out=oT_.t[:, 4 * half:4 * half + 4, :].rearrange("p (h c) t -> p h c t", h=2),
                                                                in0=bk.t[:, :].rearrange("p (h c t) -> p h c t", h=2, c=2),
                                                                in1=Ri.t[:, 2 * half:2 * half + 2, :].unsqueeze(2).to_broadcast([128, 2, 2, 128]), op=ALU.mult),
                 [bk.b, Ri.b], [oT_.b])
        yield
        for hf in range(2):
            bk = psS("C2")
            mm_group(bk, bk.t[:, :], [(oT_.t[:, c, :], w_xo.t[:, c, hf * 512:(hf + 1) * 512]) for c in range(8)], [oT_.b, w_xo.b])
            yield
            k.op(dve, lambda hf=hf, bk=bk: V_.tensor_tensor(out=X.t[:, hf * 512:(hf + 1) * 512], in0=X.t[:, hf * 512:(hf + 1) * 512], in1=bk.t[:, :], op=ALU.add),
                 [X.b, bk.b], [X.b])
        dbg_dump(X.t[:], X.b, dbg_h2 if dbg else None, i)

    def stage_D(i):
        X = Xs[i % NX]
        s2 = i % 2
        col = ncol[i % 8]
        xT, xn_ = xnTD[0], xnD[0]
        k.dma(sp, lambda: nc.sync.dma_start(out=hs_d[i * 128:(i + 1) * 128, :], in_=X.t[:]), reads=[X.b], writes=[hs_db[i]])
        yield from norm_T("D", X, xn_, xT, col, gain=gffn_b)
        b_l = psS("D")
        mm_group(b_l, b_l.t[:, 0:68], [(xT.t[:, kc, :], w_r.t[:, kc, :]) for kc in range(8)], [xT.b, w_r.b])
        yield
        lg_, r_, Mb_ = lg[s2], rt[s2], Mb[s2]
        R = r_.t
        k.op(dve, lambda: V_.tensor_tensor(out=lg_.t[:], in0=b_l.t[:, 0:68], in1=br_b.t[:], op=ALU.add), [b_l.b, br_b.b], [lg_.b])
        k.op(dve, lambda: V_.tensor_reduce(out=R[:, 0:1], in_=lg_.t[:, 64:68], axis=AX.X, op=ALU.max), [lg_.b], [r_.b])
        yield
        k.op(pool, lambda: G_.tensor_scalar(out=R[:, 1:2], in0=R[:, 0:1], scalar1=-1.0, scalar2=None, op0=ALU.mult), [r_.b], [r_.b])
        k.op(pool, lambda: G_.tensor_scalar(out=R[:, 32:64], in0=lg_.t[:, 32:64], scalar1=R[:, 1:2], scalar2=1e30, op0=ALU.add, op1=ALU.mult), [lg_.b, r_.b], [r_.b])
        k.op(pool, lambda: G_.tensor_tensor(out=R[:, 32:64], in0=R[:, 32:64], in1=lg_.t[:, 0:32], op=ALU.add), [r_.b, lg_.b], [r_.b])
        yield
        k.op(dve, lambda: V_.max(out=R[:, 4:12], in_=R[:, 32:64]), [r_.b], [r_.b])
        yield
        k.op(pool, lambda: G_.tensor_scalar(out=R[:, 64:96], in0=R[:, 32:64], scalar1=R[:, 4:5], scalar2=None, op0=ALU.is_equal), [r_.b], [r_.b])
        k.op(pool, lambda: G_.tensor_scalar(out=R[:, 96:128], in0=R[:, 32:64], scalar1=R[:, 5:6], scalar2=None, op0=ALU.is_equal), [r_.b], [r_.b])
        k.op(pool, lambda: G_.tensor_tensor(out=Mb_.t[:], in0=R[:, 64:96], in1=R[:, 96:128], op=ALU.add), [r_.b], [Mb_.b])
        k.op(pool, lambda: G_.tensor_tensor(out=R[:, 12:13], in0=R[:, 5:6], in1=R[:, 4:5], op=ALU.subtract), [r_.b], [r_.b])
        yield
        b_p = psS("D")
        k.op(pe, lambda: TE.matmul(b_p.t[:, 0:32], lhsT=slt_bf.t[:], rhs=Mb_.t[:], start=True, stop=False), [slt_bf.b, Mb_.b], [b_p.b], inc=False)
        k.op(pe, lambda: TE.matmul(b_p.t[:, 0:32], lhsT=ones_bf.t[:], rhs=Macc.t[:], start=False, stop=True), [ones_bf.b, Macc.b], [b_p.b])
        k.op(act, lambda: S_.activation(out=R[:, 20:24], in_=lg_.t[:, 64:68], func=AF.Exp, bias=R[:, 1:2], accum_out=R[:, 2:3]), [lg_.b, r_.b], [r_.b])
        k.op(act, lambda: S_.activation(out=R[:, 13:14], in_=R[:, 12:13], func=AF.Exp), [r_.b], [r_.b])
        yield
        k.op(dve, lambda: V_.scalar_tensor_tensor(out=R[:, 128:160], in0=b_p.t[:, 0:32], scalar=float(CAP - 1), in1=ecap, op0=ALU.min, op1=ALU.add), [b_p.b, cst.b], [r_.b])
        k.op(pool, lambda: G_.tensor_tensor(out=Macc.t[:], in0=Macc.t[:], in1=Mb_.t[:], op=ALU.add), [Macc.b, Mb_.b], [Macc.b])
        yield
        k.op(pool, lambda: G_.tensor_tensor(out=R[:, 64:96], in0=R[:, 64:96], in1=R[:, 128:160], op=ALU.mult), [r_.b], [r_.b])
        k.op(pool, lambda: G_.tensor_tensor(out=R[:, 96:128], in0=R[:, 96:128], in1=R[:, 128:160], op=ALU.mult), [r_.b], [r_.b])
        k.op(pool, lambda: G_.tensor_scalar(out=R[:, 14:15], in0=R[:, 13:14], scalar1=1.0, scalar2=R[:, 2:3], op0=ALU.add, op1=ALU.mult), [r_.b], [r_.b])
        k.op(pool, lambda: G_.tensor_tensor(out=gates.t[:, i, 0:1], in0=R[:, 14:15], in1=nhalf.t[:, 7:8], op=ALU.pow), [r_.b, nhalf.b], [gates_b[i]])
        k.op(pool, lambda: G_.tensor_tensor(out=gates.t[:, i, 1:2], in0=R[:, 13:14], in1=gates.t[:, i, 0:1], op=ALU.mult), [r_.b, gates_b[i]], [gates_b[i]])
        yield
        k.op(dve, lambda: V_.tensor_reduce(out=R[:, 16:18], in_=R[:, 64:128].rearrange("p (a b) -> p a b", a=2), axis=AX.X, op=ALU.add), [r_.b], [r_.b])
        k.op(dve, lambda: V_.tensor_scalar(out=dest.t[:, i, :], in0=R[:, 16:18], scalar1=float(NSLOT - 1), scalar2=None, op0=ALU.min), [r_.b], [dest_b[i]])
        yield
        for kk in range(2):
            k.dma(pool, lambda kk=kk: G_.indirect_dma_start(out=xs_d[:, :], out_offset=bass.IndirectOffsetOnAxis(ap=dest.t[:, i, kk:kk + 1], axis=0),
                                                           in_=xn_.t[:], in_offset=None),
                  reads=[xn_.b, dest_b[i]], writes=[xs_db[2 * i + kk]])

    stages = [stage_A, stage_B, stage_C, stage_C2, stage_D]
    nst = len(stages)
    prereq = {
        0: lambda i: [(0, i - 1), (1, i - 2), (4, i - 5)],
        1: lambda i: [(0, i), (1, i - 1), (2, i - 2)],
        2: lambda i: [(1, i), (2, i - 1), (3, i - 2)],
        3: lambda i: [(2, i), (3, i - 1)],
        4: lambda i: [(3, i), (4, i - 1)],
    }
    done = set()
    nxt = [0] * nst
    active = []
    while len(done) < nst * TT:
        for s_i in reversed(range(nst)):
            i_ = nxt[s_i]
            if i_ < TT and all((p[1] < 0) or (p in done) for p in prereq[s_i](i_)):
                active.append(((s_i, i_), stages[s_i](i_)))
                nxt[s_i] += 1
        prio_ = {4: 0, 3: 1, 1: 2, 2: 3, 0: 4}
        for item in sorted(active, key=lambda it: prio_[it[0][0]]):
            try:
                next(item[1])
            except StopIteration:
                active.remove(item)
                done.add(item[0])
    k.barrier()
    pop_scope()

    push_scope()
    wg = sbn("wg", [128, 8, 512], BF16, 2)
    wu = sbn("wu", [128, 8, 512], BF16, 2)
    wd = sbn("wd", [128, 4, 1024], BF16, 2)
    xsT = sbn("xsT", [128, 8, CAP], BF16, 2)
    hid = sbn("hid", [128, 4, CAP], BF16, 2)
    sgt = sbn("sgt", [128, 512], F32, 2)
    yo = sbn("yo", [128, D], BF16, 3)
    gfin_b = sb("gfin_b", [128, D], F32)
    hsT = sbn("hsT", [128, D], F32, 3)
    y0 = sbn("y0", [128, D], BF16, 3)
    fo = sbn("fo", [128, D], F32, 2)
    y1 = sbn("y1", [128, D], BF16, 3)
    fcol = sbn("fcol", [128, 2], F32, 3)
    epsc = sb("epsc", [128, 1], F32)
    k.op(pool, lambda: G_.memset(epsc.t[:], EPS), [], [epsc.b])
    fjunk = sb("fjunk", [128, D], BF16)
    k.dma(sp, lambda: nc.sync.dma_start(out=gfin_b.t[:], in_=gfin_d[0:1, :].partition_broadcast(128)), writes=[gfin_b.b])
    wdb = Buf("wdram")
    yb_db = Buf("yb")
    blocks = []
    o_ = 0
    while o_ < CAP:
        w_ = min(512, CAP - o_)
        blocks.append((o_, w_))
        o_ += w_
    yo_i = 0
    sg_i = 0
    NCH = CAP // 128
    xtok = sbn("xtok", [128, D], BF16, 3)
    xtok_i = [0]

    def exp_loads(e):
        se = e % 2
        wg_, wu_, wd_ = wg[se], wu[se], wd[se]
        k.dma(pool, lambda: G_.dma_start(out=wg_.t[:], in_=w_eg_d[e].rearrange("(kc p) f -> p kc f", p=128)), reads=[wdb], writes=[wg_.b])
        k.dma(pool, lambda: G_.dma_start(out=wu_.t[:], in_=w_eu_d[e].rearrange("(kc p) f -> p kc f", p=128)), reads=[wdb], writes=[wu_.b])
        k.dma(pool, lambda: G_.dma_start(out=wd_.t[:], in_=w_ed_d[e].rearrange("(fc p) d -> p fc d", p=128)), reads=[wdb], writes=[wd_.b])

    def exp_xpose(e, sc):
        xT_ = xsT[e % 2]
        xt_ = xtok[xtok_i[0] % 3]
        xtok_i[0] += 1
        r0 = e * CAP + sc * 128
        k.dma(sp, lambda: nc.sync.dma_start(out=xt_.t[:], in_=xs_d[r0:r0 + 128, :]), reads=[wdb], writes=[xt_.b])
        bk = ps()
        pv = bk.t[:].bitcast(BF16)
        for kc in range(8):
            k.op(pe, lambda kc=kc: TE.transpose(out=pv[:, kc * 128:(kc + 1) * 128], in_=xt_.t[:, kc * 128:(kc + 1) * 128], identity=ident.t[:]),
                 [xt_.b, ident.b], [bk.b], inc=(kc == 7))
        if sc % 2 == 0:
            k.op(act, lambda: S_.copy(out=xT_.t[:, :, sc * 128:(sc + 1) * 128], in_=pv.rearrange("p (a b) -> p a b", a=8)), [bk.b], [xT_.b])
        else:
            k.op(dve, lambda: V_.tensor_copy(out=xT_.t[:, :, sc * 128:(sc + 1) * 128], in_=pv.rearrange("p (a b) -> p a b", a=8)), [bk.b], [xT_.b])

    def exp_compute(e):
        nonlocal yo_i, sg_i
        se = e % 2
        wg_, wu_, wd_, xT_, hd_ = wg[se], wu[se], wd[se], xsT[se], hid[se]
        nxt_sc = 0
        for (o0, w0) in blocks:
            for fc in range(4):
                b_g, b_u = ps(), ps()
                mm_group(b_g, b_g.t[:, 0:w0], [(wg_.t[:, kc, fc * 128:(fc + 1) * 128], xT_.t[:, kc, o0:o0 + w0]) for kc in range(8)], [wg_.b, xT_.b])
                mm_group(b_u, b_u.t[:, 0:w0], [(wu_.t[:, kc, fc * 128:(fc + 1) * 128], xT_.t[:, kc, o0:o0 + w0]) for kc in range(8)], [wu_.b, xT_.b])
                sg_ = sgt[sg_i % 2]
                sg_i += 1
                k.op(act, lambda: S_.activation(out=sg_.t[:, 0:w0], in_=b_g.t[:, 0:w0], func=AF.Silu), [b_g.b], [sg_.b])
                k.op(dve, lambda: V_.tensor_tensor(out=hd_.t[:, fc, o0:o0 + w0], in0=sg_.t[:, 0:w0], in1=b_u.t[:, 0:w0], op=ALU.mult), [sg_.b, b_u.b], [hd_.b])
                if e + 1 < NEXP and nxt_sc < NCH:
                    exp_xpose(e + 1, nxt_sc)
                    nxt_sc += 1
        for sc in range(NCH):
            yo_ = yo[yo_i % 3]
            yo_i += 1
            for hf in range(2):
                bk = ps()
                mm_group(bk, bk.t[:, :], [(hd_.t[:, fc, sc * 128:(sc + 1) * 128], wd_.t[:, fc, hf * 512:(hf + 1) * 512]) for fc in range(4)], [hd_.b, wd_.b])
                if hf == 0:
                    k.op(act, lambda: S_.copy(out=yo_.t[:, 0:512], in_=bk.t[:, :]), [bk.b], [yo_.b])
                else:
                    k.op(dve, lambda: V_.tensor_copy(out=yo_.t[:, 512:1024], in_=bk.t[:, :]), [bk.b], [yo_.b])
            r0 = e * CAP + sc * 128
            k.dma(act, lambda: S_.dma_start(out=yb_d[r0:r0 + 128, :], in_=yo_.t[:]), reads=[yo_.b], writes=[Buf()])
        while e + 1 < NEXP and nxt_sc < NCH:
            exp_xpose(e + 1, nxt_sc)
            nxt_sc += 1

    exp_loads(0)
    for sc in range(NCH):
        exp_xpose(0, sc)
    for e in range(NEXP):
        if e + 1 < NEXP:
            exp_loads(e + 1)
        exp_compute(e)
    k.barrier()

    out_db = Buf("out")
    NF = 3

    def fin_loads(i):
        s3 = i % NF
        h_, a_, b_ = hsT[s3], y0[s3], y1[s3]
        k.dma(sp, lambda: nc.sync.dma_start(out=h_.t[:], in_=hs_d[i * 128:(i + 1) * 128, :]), reads=[wdb], writes=[h_.b])
        k.dma(pool, lambda: G_.indirect_dma_start(out=a_.t[:], out_offset=None, in_=yb_d[:, :], in_offset=bass.IndirectOffsetOnAxis(ap=dest.t[:, i, 0:1], axis=0)),
              reads=[wdb, dest_b[i]], writes=[a_.b])
        k.dma(pool, lambda: G_.indirect_dma_start(out=b_.t[:], out_offset=None, in_=yb_d[:, :], in_offset=bass.IndirectOffsetOnAxis(ap=dest.t[:, i, 1:2], axis=0)),
              reads=[wdb, dest_b[i]], writes=[b_.b])

    def fin_compute(i):
        s3 = i % NF
        h_, a_, b_, c_ = hsT[s3], y0[s3], y1[s3], fcol[s3]
        k.op(dve, lambda: V_.scalar_tensor_tensor(out=h_.t[:], in0=a_.t[:], scalar=gates.t[:, i, 0:1], in1=h_.t[:], op0=ALU.mult, op1=ALU.add),
             [a_.b, gates_b[i], h_.b], [h_.b])
        k.op(dve, lambda: V_.scalar_tensor_tensor(out=h_.t[:], in0=b_.t[:], scalar=gates.t[:, i, 1:2], in1=h_.t[:], op0=ALU.mult, op1=ALU.add),
             [b_.b, gates_b[i], h_.b], [h_.b])
        k.op(act, lambda: S_.activation(out=fjunk.t[:], in_=h_.t[:], func=AF.Square, accum_out=c_.t[:, 0:1]), [h_.b], [fjunk.b, c_.b])
        k.op(act, lambda: S_.activation(out=c_.t[:, 0:1], in_=c_.t[:, 0:1], func=AF.Sqrt, scale=1.0 / D, bias=epsc.t[:, 0:1]), [c_.b, epsc.b], [c_.b])
        k.op(dve, lambda: V_.reciprocal(out=c_.t[:, 0:1], in_=c_.t[:, 0:1]), [c_.b], [c_.b])
        fo_ = fo[i % 2]
        k.op(dve, lambda: V_.scalar_tensor_tensor(out=fo_.t[:], in0=h_.t[:], scalar=c_.t[:, 0:1], in1=gfin_b.t[:], op0=ALU.mult, op1=ALU.mult),
             [h_.b, c_.b, gfin_b.b], [fo_.b])
        k.dma(act, lambda: S_.dma_start(out=out_d[i * 128:(i + 1) * 128, :], in_=fo_.t[:]), reads=[fo_.b], writes=[out_db])

    fin_loads(0)
    if TT > 1:
        fin_loads(1)
    for i in range(TT):
        if i + 2 < TT:
            fin_loads(i + 2)
        fin_compute(i)
    k.barrier()
    pop_scope()
    pop_scope()
    k.close()
    return nc


def _consts(CAP):
    c = np.zeros((128, 674), np.float32)
    s = np.arange(128)[:, None]
    t = np.arange(128)[None, :]
    c[:, 0:128] = (s == t)
    c[:, 128:256] = (s <= t)
    c[:, 256:384] = (s < t)
    c[:, 384:512] = 1.0
    m = np.zeros((128, 130), np.float32)
    m[:, 0:128] = ((s >= 64) & (s <= t)).astype(np.float32) - ((s <= 63) & (s > t)).astype(np.float32)
    m[:, 128] = (np.arange(128) <= 63)
    m[:, 129] = 1.0
    c[:, 512:642] = m
    c[:, 642:674] = (np.arange(32) * CAP)[None, :]
    return c


def _prep_shared(inp, CAP):
    f = lambda a: np.ascontiguousarray(np.asarray(a, dtype=np.float32))
    col = lambda v: f(v).reshape(8, 128).T
    w_rg = f(inp["w_router_group"][0])
    w_re = f(inp["w_router_expert"][0])
    b_rg = f(inp["b_router_group"][0])
    b_re = f(inp["b_router_expert"][0])
    w_r = np.concatenate([w_re, np.repeat(w_rg, 8, axis=1), w_rg], axis=1)
    b_r = np.concatenate([b_re, np.repeat(b_rg, 8), b_rg])[None, :]
    gcols = np.concatenate([col(inp["norm_mix"][0]), col(inp["norm_xattn"][0]), col(inp["norm_mem"][0]),
                            col(np.concatenate([f(inp["gmlp_beta"][0]), f(inp["hgrn_out_gain"][0])]))], axis=1)
    return {
        "w_in": f(inp["w_in"][0]), "w_out": f(inp["w_out"][0]), "w_xq": f(inp["w_xq"][0]),
        "w_xkv": f(inp["w_xkv"][0]), "w_xo": f(inp["w_xo"][0]),
        "w_eg": f(inp["w_expert_gate"][0]), "w_eu": f(inp["w_expert_up"][0]), "w_ed": f(inp["w_expert_down"][0]),
        "w_r": f(w_r), "b_r": f(b_r), "gcols": f(gcols),
        "lng": f(inp["gmlp_ln"][0])[None, :], "lbl": f(inp["hgrn_lb_logits"]),
        "gffn": f(inp["norm_ffn"][0])[None, :], "gfin": f(inp["norm_final"])[None, :],
        "wspT": f(np.transpose(f(inp["gmlp_w_spatial"][0]), (2, 0, 1))),
        "bspT": f(f(inp["gmlp_b_spatial"][0]).T),
        "cst": _consts(CAP),
    }


CAP_FULL = 1152


def run(inp, n_cores, NSEQ, S, CAP, dbg=False):
    nc = build_nc(NSEQ, S, CAP, dbg=dbg)
    shared = _prep_shared(inp, CAP)
    x = np.asarray(inp["x"], dtype=np.float32)
    mem = np.asarray(inp["mem"], dtype=np.float32)
    in_maps = []
    for c in range(n_cores):
        m = dict(shared)
        m["x"] = np.ascontiguousarray(x[c * NSEQ:(c + 1) * NSEQ].reshape(NSEQ * S, D))
        m["mem"] = np.ascontiguousarray(mem[c * NSEQ:(c + 1) * NSEQ].reshape(NSEQ * 256, D))
        in_maps.append(m)
    res = run_bass_kernel_spmd(nc, in_maps, core_ids=list(range(n_cores)))
    return res


def kernel(**inputs):
    B, S, _ = inputs["x"].shape
    NSEQ = B // N_CORES
    res = run(inputs, N_CORES, NSEQ, S, CAP_FULL)
    out = np.concatenate([r["out"].reshape(NSEQ, S, D) for r in res.results], axis=0)
    return out.astype(np.float32)
```
